# Optimizing a Trainium2 kernel written in Bass

```python
import math
import jax, jax.numpy as jnp
from jax import lax
import numpy as np

D_MODEL = 4096
BATCH = 2
SEQ = 8192
DEPTH = 1
DEC_BATCH = 16
DEC_SEQ = 64
PAST_LEN = 2048

CHUNK = 64
PLE_DIM = 256
MIX_W = D_MODEL
POOL_W = MIX_W // 2
SSM_W = MIX_W - POOL_W
POOL_WINDOWS = (2, 4, 8, 16)
POOL_GROUPS = len(POOL_WINDOWS)
POOL_GROUP = POOL_W // POOL_GROUPS
POOL_BUF = max(POOL_WINDOWS) - 1
SSM_CH = 16
SSM_GROUPS = SSM_W // SSM_CH
SSM_N = 64
PEER_HEADS = 8
PEER_NKEYS = 128
PEER_EXPERTS = PEER_NKEYS * PEER_NKEYS
PEER_DK = 256
PEER_DK_HALF = PEER_DK // 2
PEER_TOPK = 16
PEER_TOK_BLOCK = 128
EPS = 1e-6

kernel_name = 'hybrid_pool_s5_peer_stream_step'


def rmsnorm(x, g):
    xf = x.astype(jnp.float32)
    y = xf * lax.rsqrt(jnp.mean(xf * xf, axis=-1, keepdims=True) + EPS) * g.astype(jnp.float32)
    return y.astype(x.dtype)


def pool_mixer(z, buf, pos0, w_pool, scale):
    bsz, L, C = z.shape
    zc = jnp.concatenate([buf.astype(z.dtype), z], axis=1)
    zf = zc.astype(jnp.float32)
    cs = jnp.concatenate([jnp.zeros((bsz, 1, C), jnp.float32), jnp.cumsum(zf, axis=1)], axis=1)
    pos = pos0 + jnp.arange(L)
    means = []
    for g, w in enumerate(POOL_WINDOWS):
        sl = slice(g * POOL_GROUP, (g + 1) * POOL_GROUP)
        s = cs[:, POOL_BUF + 1:POOL_BUF + 1 + L, sl] - cs[:, POOL_BUF + 1 - w:POOL_BUF + 1 - w + L, sl]
        cnt = jnp.minimum(pos + 1, w).astype(jnp.float32)[None, :, None]
        means.append(s / cnt)
    d = jnp.concatenate(means, axis=-1) - zf[:, POOL_BUF:]
    d = d.reshape(bsz, L, POOL_GROUPS, POOL_GROUP)
    y = jnp.einsum('blgc,gcd->blgd', d, w_pool.astype(jnp.float32)).reshape(bsz, L, POOL_W)
    y = y * scale.astype(jnp.float32)
    return y.astype(z.dtype), zc[:, -POOL_BUF:]


def ssm_mixer(u, h_re, h_im, a_re, a_im, log_dt, b_re, b_im, c_re, c_im, d_skip):
    bsz, L, _ = u.shape
    f32 = jnp.float32
    uf = u.astype(f32).reshape(bsz, L, SSM_GROUPS, SSM_CH)
    lam = lax.complex(a_re.astype(f32), a_im.astype(f32))
    dt = jnp.exp(log_dt.astype(f32))[:, None]
    lam_bar = jnp.exp(lam * dt)
    b_bar = ((lam_bar - 1.0) / lam)[..., None] * lax.complex(b_re.astype(f32), b_im.astype(f32))
    c_mat = lax.complex(c_re.astype(f32), c_im.astype(f32))
    dsk = d_skip.astype(f32)
    h0 = lax.complex(h_re.astype(f32), h_im.astype(f32))
    blk = CHUNK if L % CHUNK == 0 else L
    nb = L // blk
    ub = uf.reshape(bsz, nb, blk, SSM_GROUPS, SSM_CH).transpose(1, 0, 2, 3, 4)

    def combine(e1, e2):
        a1, s1 = e1
        a2, s2 = e2
        return a1 * a2, a2 * s1 + s2

    def step(h, u_blk):
        bu = jnp.einsum('blgi,gni->blgn', u_blk.astype(jnp.complex64), b_bar)
        bu = bu.at[:, 0].add(lam_bar[None] * h)
        a = jnp.broadcast_to(lam_bar, bu.shape)
        _, hs = lax.associative_scan(combine, (a, bu), axis=1)
        y = jnp.einsum('gin,blgn->blgi', c_mat, hs).real + dsk[None, None] * u_blk
        return hs[:, -1], y

    h_last, ys = lax.scan(step, h0, ub)
    y = ys.transpose(1, 0, 2, 3, 4).reshape(bsz, L, SSM_W)
    return y, jnp.real(h_last), jnp.imag(h_last)


def peer(xn, w_query, sub_keys, expert_u, expert_v):
    bsz, L, D = xn.shape
    T = bsz * L
    xf = xn.reshape(T, D)
    q = (xf @ w_query).astype(jnp.float32).reshape(T, PEER_HEADS, 2, PEER_DK_HALF)
    s = jnp.einsum('thpk,hpnk->thpn', q, sub_keys.astype(jnp.float32))
    s1, i1 = lax.top_k(s[:, :, 0], PEER_TOPK)
    s2, i2 = lax.top_k(s[:, :, 1], PEER_TOPK)
    cand = (s1[..., :, None] + s2[..., None, :]).reshape(T, PEER_HEADS, PEER_TOPK * PEER_TOPK)
    cidx = (i1[..., :, None] * PEER_NKEYS + i2[..., None, :]).reshape(T, PEER_HEADS, PEER_TOPK * PEER_TOPK)
    top, sel = lax.top_k(cand, PEER_TOPK)
    idx = jnp.take_along_axis(cidx, sel, axis=-1)
    gates = jax.nn.softmax(top, axis=-1)
    nblk = -(-T // PEER_TOK_BLOCK)
    pad = nblk * PEER_TOK_BLOCK - T
    xr = jnp.pad(xf, ((0, pad), (0, 0))).reshape(nblk, PEER_TOK_BLOCK, D)
    ir = jnp.pad(idx, ((0, pad), (0, 0), (0, 0))).reshape(nblk, PEER_TOK_BLOCK, PEER_HEADS, PEER_TOPK)
    gr = jnp.pad(gates, ((0, pad), (0, 0), (0, 0))).reshape(nblk, PEER_TOK_BLOCK, PEER_HEADS, PEER_TOPK)

    def block(args):
        xb, ib, gb = args
        u = expert_u[ib]
        hpre = jnp.einsum('td,thkd->thk', xb, u).astype(jnp.float32)
        act = (jax.nn.gelu(hpre, approximate=False) * gb).astype(xb.dtype)
        v = expert_v[ib]
        return jnp.einsum('thk,thkd->td', act, v)

    out = lax.map(block, (xr, ir, gr)).reshape(nblk * PEER_TOK_BLOCK, D)[:T]
    return out.reshape(bsz, L, D).astype(xn.dtype)


def layer(x, p, pool_buf, h_re, h_im, pos0, g_mix, w_in, w_pool, pool_scale, a_re, a_im, log_dt,
          b_re, b_im, c_re, c_im, d_skip, w_glu, b_glu, w_out, g_ffn, w_query, sub_keys,
          expert_u, expert_v, g_ple, w_ple_gate, w_ple):
    h = rmsnorm(x, g_mix)
    z = h @ w_in
    y_pool, new_buf = pool_mixer(z[..., :POOL_W], pool_buf, pos0, w_pool, pool_scale)
    y_ssm, new_re, new_im = ssm_mixer(z[..., POOL_W:], h_re, h_im, a_re, a_im, log_dt,
                                      b_re, b_im, c_re, c_im, d_skip)
    s = jax.nn.gelu(y_ssm, approximate=False)
    s = s * jax.nn.sigmoid(s @ w_glu.astype(jnp.float32) + b_glu.astype(jnp.float32))
    mixed = jnp.concatenate([y_pool, s.astype(y_pool.dtype)], axis=-1)
    x = x + (mixed @ w_out).astype(x.dtype)
    x = x + peer(rmsnorm(x, g_ffn), w_query, sub_keys, expert_u, expert_v)
    gate = jax.nn.sigmoid((rmsnorm(x, g_ple) @ w_ple_gate).astype(jnp.float32))
    x = x + ((p.astype(jnp.float32) @ w_ple.astype(jnp.float32)) * gate).astype(x.dtype)
    return x, new_buf, new_re, new_im


def setup_inputs(seed: int = 0) -> dict:
    key = jax.random.key(seed)
    ks = jax.random.split(key, 32)
    f32 = jnp.float32

    def nrm(k, shape, scale):
        return scale * jax.random.normal(k, shape, f32)

    L = DEPTH
    G, N = SSM_GROUPS, SSM_N
    return {
        'x_prompt': nrm(ks[0], (BATCH, SEQ, D_MODEL), 1.0),
        'x_sample': nrm(ks[1], (DEC_BATCH, DEC_SEQ, D_MODEL), 1.0),
        'p_prompt': nrm(ks[2], (L, BATCH, SEQ, PLE_DIM), 1.0),
        'p_sample': nrm(ks[3], (L, DEC_BATCH, DEC_SEQ, PLE_DIM), 1.0),
        'cache_pool': nrm(ks[4], (L, DEC_BATCH, POOL_BUF, POOL_W), 1.0),
        'state_ssm_re': nrm(ks[5], (L, DEC_BATCH, G, N), 0.5),
        'state_ssm_im': nrm(ks[6], (L, DEC_BATCH, G, N), 0.5),
        'g_mix': 1.0 + nrm(ks[7], (L, D_MODEL), 0.02),
        'w_in': nrm(ks[8], (L, D_MODEL, MIX_W), D_MODEL ** -0.5),
        'w_pool': nrm(ks[9], (L, POOL_GROUPS, POOL_GROUP, POOL_GROUP), POOL_GROUP ** -0.5),
        'pool_scale': 1.0 + nrm(ks[10], (L, POOL_W), 0.02),
        'ssm_a_re': -0.5 + nrm(ks[11], (L, G, N), 0.01),
        'ssm_a_im': math.pi * jnp.arange(N, dtype=f32) + nrm(ks[12], (L, G, N), 0.01),
        'ssm_log_dt': jax.random.uniform(ks[13], (L, G), f32, math.log(1e-3), math.log(1e-1)),
        'ssm_b_re': nrm(ks[14], (L, G, N, SSM_CH), (2 * SSM_CH) ** -0.5),
        'ssm_b_im': nrm(ks[15], (L, G, N, SSM_CH), (2 * SSM_CH) ** -0.5),
        'ssm_c_re': nrm(ks[16], (L, G, SSM_CH, N), N ** -0.5),
        'ssm_c_im': nrm(ks[17], (L, G, SSM_CH, N), N ** -0.5),
        'ssm_d': nrm(ks[18], (L, G, SSM_CH), 1.0),
        'w_glu': nrm(ks[19], (L, SSM_W, SSM_W), SSM_W ** -0.5),
        'b_glu': nrm(ks[20], (L, SSM_W), 0.02),
        'w_out': nrm(ks[21], (L, MIX_W, D_MODEL), MIX_W ** -0.5),
        'g_ffn': 1.0 + nrm(ks[22], (L, D_MODEL), 0.02),
        'w_query': nrm(ks[23], (L, D_MODEL, PEER_HEADS * PEER_DK), D_MODEL ** -0.5),
        'peer_sub_keys': nrm(ks[24], (L, PEER_HEADS, 2, PEER_NKEYS, PEER_DK_HALF), PEER_DK_HALF ** -0.5),
        'expert_u': nrm(ks[25], (L, PEER_EXPERTS, D_MODEL), D_MODEL ** -0.5),
        'expert_v': nrm(ks[26], (L, PEER_EXPERTS, D_MODEL), PEER_HEADS ** -0.5),
        'g_ple': 1.0 + nrm(ks[27], (L, D_MODEL), 0.02),
        'w_ple_gate': nrm(ks[28], (L, D_MODEL, D_MODEL), D_MODEL ** -0.5),
        'w_ple': nrm(ks[29], (L, PLE_DIM, D_MODEL), PLE_DIM ** -0.5),
        'g_final': 1.0 + nrm(ks[30], (D_MODEL,), 0.02),
    }


def reference(x_prompt, x_sample, p_prompt, p_sample, cache_pool, state_ssm_re, state_ssm_im,
              g_mix, w_in, w_pool, pool_scale, ssm_a_re, ssm_a_im, ssm_log_dt, ssm_b_re, ssm_b_im,
              ssm_c_re, ssm_c_im, ssm_d, w_glu, b_glu, w_out, g_ffn, w_query, peer_sub_keys,
              expert_u, expert_v, g_ple, w_ple_gate, w_ple, g_final):
    bp = x_prompt.shape[0]
    xp = x_prompt
    xs = x_sample
    pool_p, re_p, im_p, pool_s, re_s, im_s = [], [], [], [], [], []
    for i in range(DEPTH):
        lw = (g_mix[i], w_in[i], w_pool[i], pool_scale[i], ssm_a_re[i], ssm_a_im[i], ssm_log_dt[i],
              ssm_b_re[i], ssm_b_im[i], ssm_c_re[i], ssm_c_im[i], ssm_d[i], w_glu[i], b_glu[i],
              w_out[i], g_ffn[i], w_query[i], peer_sub_keys[i], expert_u[i], expert_v[i],
              g_ple[i], w_ple_gate[i], w_ple[i])
        buf0 = jnp.zeros((bp, POOL_BUF, POOL_W), xp.dtype)
        h0 = jnp.zeros((bp, SSM_GROUPS, SSM_N), jnp.float32)
        xp, nb_p, nr_p, ni_p = layer(xp, p_prompt[i], buf0, h0, h0, 0, *lw)
        xs, nb_s, nr_s, ni_s = layer(xs, p_sample[i], cache_pool[i], state_ssm_re[i], state_ssm_im[i],
                                     PAST_LEN, *lw)
        pool_p.append(nb_p)
        re_p.append(nr_p)
        im_p.append(ni_p)
        pool_s.append(nb_s)
        re_s.append(nr_s)
        im_s.append(ni_s)
    y_prompt = rmsnorm(xp, g_final)
    y_sample = rmsnorm(xs, g_final)
    new_pool_prompt = jnp.stack(pool_p)
    new_ssm_re_prompt = jnp.stack(re_p)
    new_ssm_im_prompt = jnp.stack(im_p)
    new_pool_sample = jnp.stack(pool_s)
    new_ssm_re_sample = jnp.stack(re_s)
    new_ssm_im_sample = jnp.stack(im_s)
    return (y_prompt, y_sample, new_pool_prompt, new_ssm_re_prompt, new_ssm_im_prompt,
            new_pool_sample, new_ssm_re_sample, new_ssm_im_sample)
```

```python
import math
import numpy as np
from contextlib import ExitStack
import concourse.bass as bass
import concourse.mybir as mybir
from concourse.bass_utils import run_bass_kernel_spmd

F32 = mybir.dt.float32
BF16 = mybir.dt.bfloat16
I32 = mybir.dt.int32
AF = mybir.ActivationFunctionType
ALU = mybir.AluOpType
AX = mybir.AxisListType

NCORES = 8
D = 4096
SEG = 2048
NSAMP = 128
NTOK = SEG + NSAMP
NT = 256
PRE = 3 * SEG
LC = 16
LB = 4
REV_AP = True
NOSYNC = True
EPS = 1e-6
NLAM = 64 + 2048
TWO_PI = 2.0 * math.pi


class SemObj:
    def __init__(self, h):
        self.h = h
        self.total = 0


class Eng:
    def __init__(self, k, name, obj):
        self.name = name
        self.obj = obj
        self.sem = SemObj(k.es.enter_context(k.nc.semaphore("s_" + name)))
        self.count = 0
        self.seen = {}


class Buf:
    __slots__ = ("w", "r")

    def __init__(self):
        self.w = None
        self.r = {}


def bufs(n):
    return [Buf() for _ in range(n)]


class KB:
    def __init__(self):
        self.nc = bass.Bass("TRN2", target_bir_lowering=False)
        self.es = ExitStack()
        nc = self.nc
        self.pe = Eng(self, "pe", nc.tensor)
        self.act = Eng(self, "act", nc.scalar)
        self.dve = Eng(self, "dve", nc.vector)
        self.pool = Eng(self, "pool", nc.gpsimd)
        self.sp = Eng(self, "sp", nc.sync)
        self.engs = [self.pe, self.act, self.dve, self.pool, self.sp]
        self.gsems = [SemObj(self.es.enter_context(nc.semaphore("g%d" % i))) for i in range(8)]
        self.gi = 0
        self.allsems = [e.sem for e in self.engs] + list(self.gsems)
        self.ps = []
        for i in range(8):
            t = self.es.enter_context(nc.psum_tensor("ps%d" % i, [128, 512], F32))
            self.ps.append((t, Buf()))
        self.pi = 0
        self.NS = 8
        self.wslots = []
        self.wi = 0
        self.csems = [SemObj(self.es.enter_context(nc.semaphore("c%d" % i))) for i in range(16)]
        self.allsems += self.csems
        self.ci = 0

    def init_slots(self):
        nc = self.nc
        for i in range(self.NS):
            t = self.es.enter_context(nc.sbuf_tensor("wsl%d" % i, [128, 2048], BF16))
            so = SemObj(self.es.enter_context(nc.semaphore("w%d" % i)))
            self.allsems.append(so)
            self.wslots.append((t, Buf(), so))

    def _waits(self, eng, reads, writes, extra=None, skip_self=False):
        deps = {}

        def add(so, v):
            if deps.get(so, 0) < v:
                deps[so] = v

        for b in reads:
            if b.w is not None:
                add(*b.w)
        for b in writes:
            if b.w is not None:
                add(*b.w)
            for so, v in b.r.items():
                add(so, v)
        if extra:
            for so, v in extra:
                add(so, v)
        for so, v in deps.items():
            if so is eng.sem and (eng is self.pe or skip_self):
                continue
            if eng.seen.get(so, 0) >= v:
                continue
            eng.obj.wait_ge(so.h, v)
            eng.seen[so] = v

    def _mark(self, d, reads, writes):
        so, v = d
        for b in reads:
            if b.r.get(so, 0) < v:
                b.r[so] = v
        for b in writes:
            b.w = d
            b.r = {}

    def op(self, eng, fn, reads=(), writes=(), skip_self=False):
        self._waits(eng, reads, writes, skip_self=skip_self)
        ins = fn()
        eng.count += 1
        ins.then_inc(eng.sem.h, 1)
        self._mark((eng.sem, eng.count), reads, writes)

    def dma(self, q, out, in_, reads=(), writes=(), so=None):
        if so is None:
            so = self.gsems[self.gi % len(self.gsems)]
            self.gi += 1
        extra = [(so, so.total)] if so.total > 0 else None
        self._waits(q, reads, writes, extra)
        ins = q.obj.dma_start(out=out, in_=in_)
        so.total += 16
        ins.then_inc(so.h, 16)
        self._mark((so, so.total), reads, writes)

    def barrier(self):
        for e in self.engs:
            for so in self.allsems:
                v = so.total if so.total > 0 else 0
                for f in self.engs:
                    if f.sem is so:
                        v = f.count
                if v > 0 and e.seen.get(so, 0) < v and not (so is e.sem):
                    e.obj.wait_ge(so.h, v)
                    e.seen[so] = v

    def psum(self):
        r = self.ps[self.pi % 8]
        self.pi += 1
        return r

    def sb(self, stack, name, shape, dt):
        self.uid = getattr(self, "uid", 0) + 1
        return stack.enter_context(self.nc.sbuf_tensor("sb_%s_%d" % (name, self.uid), shape, dt))

    def wload(self, src_ap, n3):
        t, b, so = self.wslots[self.wi % self.NS]
        self.wi += 1
        a, bb = n3
        dst = t[:, 0:a * bb].rearrange("p (a b) -> p a b", a=a, b=bb)
        q = self.sp if src_ap.dtype == BF16 else self.pool
        self.dma(q, dst, src_ap, reads=(), writes=(b,), so=so)
        return t, b

    def stream(self, items, LA=4):
        n = len(items)
        loaded = []
        for i in range(min(LA, n)):
            loaded.append(self.wload(items[i][0], items[i][1]))
        for i in range(n):
            wt, wb = loaded[i]
            items[i][2](wt, wb)
            if i + LA < n:
                loaded.append(self.wload(items[i + LA][0], items[i + LA][1]))

    def linear(self, wd, KC, KBk, mgs, act_in, N, evac):
        nc = self.nc
        KG = KC // KBk
        items = []
        st = {}
        for mg in mgs:
            for kg in range(KG):
                def consume(wt, wb, mg=mg, kg=kg):
                    if kg == 0:
                        st["ps"] = [self.psum() for _ in range(4)]
                    for kb in range(KBk):
                        kc = kg * KBk + kb
                        a_ap, a_buf = act_in(kc)
                        for mi in range(4):
                            pt, pb = st["ps"][mi]
                            self.op(self.pe, lambda pt=pt, kb=kb, mi=mi, a_ap=a_ap, kc=kc: nc.tensor.matmul(
                                pt[:, 0:N], lhsT=wt[:, kb * 512 + mi * 128: kb * 512 + (mi + 1) * 128], rhs=a_ap,
                                start=(kc == 0), stop=(kc == KC - 1)), reads=(wb, a_buf), writes=(pb,))
                    if kg == KG - 1:
                        for mi in range(4):
                            pt, pb = st["ps"][mi]
                            evac(mg * 4 + mi, pt, pb)
                items.append((wd[mg, kg], (KBk, 512), consume))
        self.stream(items)


def build_program():
    k = KB()
    nc = k.nc
    es = k.es
    pe, act, dve, pool, sp = k.pe, k.act, k.dve, k.pool, k.sp

    def din(name, shape):
        return nc.dram_tensor(name, list(shape), F32, kind="ExternalInput").ap()

    def dout(name, shape):
        return nc.dram_tensor(name, list(shape), F32, kind="ExternalOutput").ap()

    xT_d = din("xT", [32, 128, NTOK])
    xpre_d = din("xpreT", [32, 128, PRE])
    pT_d = din("pT", [2, 128, NTOK])
    cp_d = din("cpT", [2, 128, 16, 16])
    st0_d = din("st0", [2, 128, 2, 64])
    rcorr_d = din("rcorr", [128, 4, 16])
    vecs_d = din("vecs", [128, 176])
    ident_d = din("ident", [128, 128])
    rmask_d = din("rmask", [128, 4])
    lamp_d = din("lamp", [128, 3, NLAM])
    bq_d = din("bq", [2, 128, 16, 128])
    cz_d = din("cz", [2, 128, 64, 128])
    keys_d = din("keysT", [128, 16, 128])
    w_in_d = din("w_in_b", [8, 8, 128, 4, 512])
    w_pool_d = din("w_pool_b", [4, 1, 128, 4, 512])
    w_glu_d = din("w_glu_b", [4, 4, 128, 4, 512])
    w_out_d = din("w_out_b", [8, 8, 128, 4, 512])
    w_q_d = din("w_q_b", [4, 8, 128, 4, 512])
    w_pg_d = din("w_pg_b", [8, 8, 128, 4, 512])
    w_ple_d = din("w_ple_b", [8, 1, 128, 2, 512])
    ut_d = din("UTb", [128, 2, 128, 16, 128])
    v_d = din("Vn", [128, 128, 4096])
    conv_jobs = []

    def half(name, src):
        shp = list(src.shape)
        dst = nc.dram_tensor(name + "_h", shp, BF16, kind="Internal").ap()
        letters = "abcdefg"[:len(shp)]
        pat = " ".join(letters) + " -> (" + " ".join(letters) + ")"
        sf = src.rearrange(pat).rearrange("(n p a b) -> n p a b", p=128, a=2, b=2048)
        df = dst.rearrange(pat).rearrange("(n p a b) -> n p a b", p=128, a=2, b=2048)
        for i in range(sf.shape[0]):
            conv_jobs.append((sf[i], df[i]))
        return dst

    w_in_h = half("w_in_b", w_in_d)
    w_pool_h = half("w_pool_b", w_pool_d)
    w_glu_h = half("w_glu_b", w_glu_d)
    w_out_h = half("w_out_b", w_out_d)
    w_q_h = half("w_q_b", w_q_d)
    ut_h = half("UTb", ut_d)
    v_h = half("Vn", v_d)
    w_pg_h = half("w_pg_b", w_pg_d)
    w_ple_h = half("w_ple_b", w_ple_d)
    cstate = {"i": 0}

    def conv_pump(n):
        while n > 0 and cstate["i"] < len(conv_jobs):
            sj, dj = conv_jobs[cstate["i"]]
            cstate["i"] += 1
            n -= 1
            so = k.csems[k.ci % len(k.csems)]
            k.ci += 1
            k.dma(pool, dj, sj, so=so)

    yT_d = dout("yT", [32, 128, NTOK])
    pool_o = dout("pool_o", [3, 128, 16, 16])
    ssm_o = dout("ssm_o", [3, 128, 2, 64])

    sb = k.sb
    vecs = sb(es, "vecs", [128, 176], F32)
    vB = Buf()
    ident = sb(es, "ident", [128, 128], F32)
    idB = Buf()
    ones = sb(es, "ones", [128, 128], F32)
    onB = Buf()
    rmask = sb(es, "rmask", [128, 4], F32)
    rmB = Buf()
    rcorr = sb(es, "rcorr", [128, 4, 16], F32)
    rcB = Buf()
    sq = [sb(es, "sq%d" % i, [128, NT], F32) for i in range(2)]
    sqB = bufs(2)
    rstd = sb(es, "rstd", [128, NT], F32)
    rsB = Buf()
    halo = sb(es, "halo", [128, 16, 16], F32)
    haB = Buf()
    hcar = sb(es, "hcar", [128, 2, 64], F32)
    hcB = [Buf(), Buf()]
    LRR = sb(es, "LRR", [128, 2, 64], F32)
    LI2 = sb(es, "LI2", [128, 2, 64], F32)
    LRRb = sb(es, "LRRb", [128, 2, 64], F32)
    LI2b = sb(es, "LI2b", [128, 2, 64], F32)
    lamB = Buf()
    Bq = sb(es, "Bq", [128, 16, 2, 128], BF16)
    BqB = Buf()
    Cz = sb(es, "Cz", [128, 64, 2, 128], BF16)
    CzB = Buf()
    Dz = sb(es, "Dz", [128, 16, 128], BF16)
    DzB = Buf()
    epsT = sb(es, "epsT", [128, 1], F32)
    epB = Buf()
    GM, GF, GP, GL, PSC, BGL, DSK = 0, 32, 64, 96, 128, 144, 160

    k.dma(sp, vecs[:], vecs_d[:, :], writes=(vB,))
    k.dma(sp, ident[:], ident_d[:, :], writes=(idB,))
    k.dma(sp, rmask[:], rmask_d[:, :], writes=(rmB,))
    k.dma(sp, rcorr[:], rcorr_d[:, :, :], writes=(rcB,))
    k.op(dve, lambda: nc.vector.memset(ones[:], 1.0), writes=(onB,))
    k.op(dve, lambda: nc.vector.memset(epsT[:], EPS), writes=(epB,))
    k.op(dve, lambda: nc.vector.memset(halo[:], 0.0), writes=(haB,))
    k.op(dve, lambda: nc.vector.memset(hcar[:], 0.0), writes=(hcB[0], hcB[1]))

    with ExitStack() as ph:
        lp = sb(ph, "lp", [128, 3, NLAM], F32)
        lpB = Buf()
        k.dma(sp, lp[:], lamp_d[:, :, :], writes=(lpB,))
        W = NLAM
        tl = [sb(ph, "tl%d" % i, [128, W], F32) for i in range(8)]
        ti = sb(ph, "ti", [128, W], I32)
        tB = bufs(9)
        dtT, erT, thT, lrT, liT, t5, t6, t7 = tl
        k.op(act, lambda: nc.scalar.activation(out=dtT[:], in_=lp[:, 2, :], func=AF.Exp), reads=(lpB,), writes=(tB[0],))
        k.op(dve, lambda: nc.vector.tensor_tensor(out=erT[:], in0=lp[:, 0, :], in1=dtT[:], op=ALU.mult), reads=(lpB, tB[0]), writes=(tB[1],))
        k.op(act, lambda: nc.scalar.activation(out=erT[:], in_=erT[:], func=AF.Exp), reads=(tB[1],), writes=(tB[1],))
        k.op(dve, lambda: nc.vector.tensor_tensor(out=thT[:], in0=lp[:, 1, :], in1=dtT[:], op=ALU.mult), reads=(lpB, tB[0]), writes=(tB[2],))

        def sin_rr(dst, dB, shift):
            k.op(dve, lambda: nc.vector.tensor_scalar(out=t5[:], in0=thT[:], scalar1=shift, scalar2=None, op0=ALU.add), reads=(tB[2],), writes=(tB[5],))
            k.op(dve, lambda: nc.vector.tensor_scalar(out=t6[:], in0=t5[:], scalar1=1.0 / TWO_PI, scalar2=0.5, op0=ALU.mult, op1=ALU.add), reads=(tB[5],), writes=(tB[6],))
            k.op(dve, lambda: nc.vector.tensor_copy(out=ti[:], in_=t6[:]), reads=(tB[6],), writes=(tB[8],))
            k.op(dve, lambda: nc.vector.tensor_copy(out=t6[:], in_=ti[:]), reads=(tB[8],), writes=(tB[6],))
            k.op(dve, lambda: nc.vector.scalar_tensor_tensor(out=t5[:], in0=t6[:], scalar=-TWO_PI, in1=t5[:], op0=ALU.mult, op1=ALU.add), reads=(tB[6], tB[5]), writes=(tB[5],))
            k.op(dve, lambda: nc.vector.tensor_scalar(out=t6[:], in0=t5[:], scalar1=-math.pi, scalar2=TWO_PI, op0=ALU.is_lt, op1=ALU.mult), reads=(tB[5],), writes=(tB[6],))
            k.op(dve, lambda: nc.vector.tensor_tensor(out=t5[:], in0=t5[:], in1=t6[:], op=ALU.add), reads=(tB[5], tB[6]), writes=(tB[5],))
            k.op(dve, lambda: nc.vector.tensor_scalar(out=t6[:], in0=t5[:], scalar1=math.pi, scalar2=-TWO_PI, op0=ALU.is_gt, op1=ALU.mult), reads=(tB[5],), writes=(tB[6],))
            k.op(dve, lambda: nc.vector.tensor_tensor(out=t5[:], in0=t5[:], in1=t6[:], op=ALU.add), reads=(tB[5], tB[6]), writes=(tB[5],))
            k.op(dve, lambda: nc.vector.tensor_scalar(out=t5[:], in0=t5[:], scalar1=-3.14159, scalar2=3.14159, op0=ALU.max, op1=ALU.min), reads=(tB[5],), writes=(tB[5],))
            k.op(act, lambda: nc.scalar.activation(out=dst[:], in_=t5[:], func=AF.Sin), reads=(tB[5],), writes=(dB,))

        sin_rr(liT, tB[4], 0.0)
        sin_rr(lrT, tB[3], math.pi / 2)
        k.op(dve, lambda: nc.vector.tensor_tensor(out=liT[:], in0=liT[:], in1=erT[:], op=ALU.mult), reads=(tB[4], tB[1]), writes=(tB[4],))
        k.op(dve, lambda: nc.vector.tensor_tensor(out=lrT[:], in0=lrT[:], in1=erT[:], op=ALU.mult), reads=(tB[3], tB[1]), writes=(tB[3],))
        k.op(dve, lambda: nc.vector.tensor_copy(out=LRR[:, 0, :], in_=lrT[:, 0:64]), reads=(tB[3],), writes=(lamB,))
        k.op(dve, lambda: nc.vector.tensor_copy(out=LRR[:, 1, :], in_=lrT[:, 0:64]), reads=(tB[3],), writes=(lamB,))
        k.op(dve, lambda: nc.vector.tensor_scalar(out=LI2[:, 0, :], in0=liT[:, 0:64], scalar1=-1.0, scalar2=None, op0=ALU.mult), reads=(tB[4],), writes=(lamB,))
        k.op(dve, lambda: nc.vector.tensor_copy(out=LI2[:, 1, :], in_=liT[:, 0:64]), reads=(tB[4],), writes=(lamB,))
        k.op(dve, lambda: nc.vector.tensor_scalar(out=t5[:], in0=lrT[:], scalar1=-1.0, scalar2=None, op0=ALU.add), reads=(tB[3],), writes=(tB[5],))
        k.op(dve, lambda: nc.vector.tensor_tensor(out=t6[:], in0=lp[:, 0, :], in1=lp[:, 0, :], op=ALU.mult), reads=(lpB,), writes=(tB[6],))
        k.op(dve, lambda: nc.vector.tensor_tensor(out=t7[:], in0=lp[:, 1, :], in1=lp[:, 1, :], op=ALU.mult), reads=(lpB,), writes=(tB[7],))
        k.op(dve, lambda: nc.vector.tensor_tensor(out=t6[:], in0=t6[:], in1=t7[:], op=ALU.add), reads=(tB[6], tB[7]), writes=(tB[6],))
        k.op(dve, lambda: nc.vector.reciprocal(out=t6[:], in_=t6[:]), reads=(tB[6],), writes=(tB[6],))
        k.op(dve, lambda: nc.vector.tensor_tensor(out=dtT[:], in0=t5[:], in1=lp[:, 0, :], op=ALU.mult), reads=(tB[5], lpB), writes=(tB[0],))
        k.op(dve, lambda: nc.vector.tensor_tensor(out=t7[:], in0=liT[:], in1=lp[:, 1, :], op=ALU.mult), reads=(tB[4], lpB), writes=(tB[7],))
        k.op(dve, lambda: nc.vector.tensor_tensor(out=dtT[:], in0=dtT[:], in1=t7[:], op=ALU.add), reads=(tB[0], tB[7]), writes=(tB[0],))
        k.op(dve, lambda: nc.vector.tensor_tensor(out=dtT[:], in0=dtT[:], in1=t6[:], op=ALU.mult), reads=(tB[0], tB[6]), writes=(tB[0],))
        k.op(dve, lambda: nc.vector.tensor_tensor(out=erT[:], in0=liT[:], in1=lp[:, 0, :], op=ALU.mult), reads=(tB[4], lpB), writes=(tB[1],))
        k.op(dve, lambda: nc.vector.tensor_tensor(out=t7[:], in0=t5[:], in1=lp[:, 1, :], op=ALU.mult), reads=(tB[5], lpB), writes=(tB[7],))
        k.op(dve, lambda: nc.vector.tensor_tensor(out=erT[:], in0=erT[:], in1=t7[:], op=ALU.subtract), reads=(tB[1], tB[7]), writes=(tB[1],))
        k.op(dve, lambda: nc.vector.tensor_tensor(out=erT[:], in0=erT[:], in1=t6[:], op=ALU.mult), reads=(tB[1], tB[6]), writes=(tB[1],))
        bqr = sb(ph, "bqr", [128, 2048], F32)
        bqi = sb(ph, "bqi", [128, 2048], F32)
        bqB = bufs(2)
        k.dma(sp, bqr[:], bq_d[0].rearrange("p a b -> p (a b)"), writes=(bqB[0],))
        k.dma(sp, bqi[:], bq_d[1].rearrange("p a b -> p (a b)"), writes=(bqB[1],))
        cr = dtT[:, 64:NLAM]
        ci = erT[:, 64:NLAM]
        a5 = t5[:, 0:2048]
        a7 = t7[:, 0:2048]
        Bre = thT[:, 0:2048]
        Bim = t6[:, 0:2048]
        r3 = lambda ap: ap.rearrange("p (a b) -> p a b", a=16, b=128)
        k.op(dve, lambda: nc.vector.tensor_tensor(out=a5, in0=cr, in1=bqr[:], op=ALU.mult), reads=(tB[0], bqB[0]), writes=(tB[5],))
        k.op(dve, lambda: nc.vector.tensor_tensor(out=a7, in0=ci, in1=bqi[:], op=ALU.mult), reads=(tB[1], bqB[1]), writes=(tB[7],))
        k.op(dve, lambda: nc.vector.tensor_tensor(out=Bre, in0=a5, in1=a7, op=ALU.subtract), reads=(tB[5], tB[7]), writes=(tB[2],))
        k.op(dve, lambda: nc.vector.tensor_tensor(out=a5, in0=cr, in1=bqi[:], op=ALU.mult), reads=(tB[0], bqB[1]), writes=(tB[5],))
        k.op(dve, lambda: nc.vector.tensor_tensor(out=a7, in0=ci, in1=bqr[:], op=ALU.mult), reads=(tB[1], bqB[0]), writes=(tB[7],))
        k.op(dve, lambda: nc.vector.tensor_tensor(out=Bim, in0=a5, in1=a7, op=ALU.add), reads=(tB[5], tB[7]), writes=(tB[6],))
        k.op(dve, lambda: nc.vector.tensor_copy(out=Bq[:, :, 0, :], in_=r3(Bre)), reads=(tB[2],), writes=(BqB,))
        k.op(dve, lambda: nc.vector.tensor_copy(out=Bq[:, :, 1, :], in_=r3(Bim)), reads=(tB[6],), writes=(BqB,))
        BqP = Cz[:].rearrange("p g r m -> p (g r m)").rearrange("p (a f r m) -> p a f r m", a=LB, f=16, r=2, m=128)
        Pre, Pim, tA_ = lp[:, 0, :], lp[:, 1, :], lp[:, 2, :]
        pwB = bufs(3)
        k.op(dve, lambda: nc.vector.memset(Pre, 1.0), reads=(tB[0], tB[1]), writes=(pwB[0], lpB))
        k.op(dve, lambda: nc.vector.memset(Pim, 0.0), writes=(pwB[1], lpB))
        for e_ in range(LB):
            a_ = LB - 1 - e_
            k.op(dve, lambda: nc.vector.tensor_tensor(out=a5, in0=Pre[:, 64:NLAM], in1=Bre, op=ALU.mult), reads=(pwB[0], tB[2]), writes=(tB[5],))
            k.op(dve, lambda: nc.vector.tensor_tensor(out=a7, in0=Pim[:, 64:NLAM], in1=Bim, op=ALU.mult), reads=(pwB[1], tB[6]), writes=(tB[7],))
            k.op(dve, lambda a_=a_: nc.vector.tensor_tensor(out=BqP[:, a_, :, 0, :], in0=r3(a5), in1=r3(a7), op=ALU.subtract), reads=(tB[5], tB[7]), writes=(CzB,))
            k.op(dve, lambda: nc.vector.tensor_tensor(out=a5, in0=Pre[:, 64:NLAM], in1=Bim, op=ALU.mult), reads=(pwB[0], tB[6], CzB), writes=(tB[5],))
            k.op(dve, lambda: nc.vector.tensor_tensor(out=a7, in0=Pim[:, 64:NLAM], in1=Bre, op=ALU.mult), reads=(pwB[1], tB[2], CzB), writes=(tB[7],))
            k.op(dve, lambda a_=a_: nc.vector.tensor_tensor(out=BqP[:, a_, :, 1, :], in0=r3(a5), in1=r3(a7), op=ALU.add), reads=(tB[5], tB[7]), writes=(CzB,))
            k.op(dve, lambda: nc.vector.tensor_tensor(out=tA_, in0=Pre, in1=lrT[:], op=ALU.mult), reads=(pwB[0], tB[3]), writes=(pwB[2],))
            k.op(dve, lambda: nc.vector.tensor_tensor(out=t5[:], in0=Pim, in1=liT[:], op=ALU.mult), reads=(pwB[1], tB[4], CzB), writes=(tB[5],))
            k.op(dve, lambda: nc.vector.tensor_tensor(out=tA_, in0=tA_, in1=t5[:], op=ALU.subtract), reads=(pwB[2], tB[5]), writes=(pwB[2],))
            k.op(dve, lambda: nc.vector.tensor_tensor(out=t5[:], in0=Pre, in1=liT[:], op=ALU.mult), reads=(pwB[0], tB[4], pwB[2]), writes=(tB[5],))
            k.op(dve, lambda: nc.vector.tensor_tensor(out=t7[:], in0=Pim, in1=lrT[:], op=ALU.mult), reads=(pwB[1], tB[3], CzB), writes=(tB[7],))
            k.op(dve, lambda: nc.vector.tensor_tensor(out=Pim, in0=t5[:], in1=t7[:], op=ALU.add), reads=(tB[5], tB[7]), writes=(pwB[1],))
            k.op(dve, lambda: nc.vector.tensor_copy(out=Pre, in_=tA_), reads=(pwB[2],), writes=(pwB[0],))
        k.op(dve, lambda: nc.vector.tensor_copy(out=LRRb[:, 0, :], in_=Pre[:, 0:64]), reads=(pwB[0],), writes=(lamB,))
        k.op(dve, lambda: nc.vector.tensor_copy(out=LRRb[:, 1, :], in_=Pre[:, 0:64]), reads=(pwB[0],), writes=(lamB,))
        k.op(dve, lambda: nc.vector.tensor_scalar(out=LI2b[:, 0, :], in0=Pim[:, 0:64], scalar1=-1.0, scalar2=None, op0=ALU.mult), reads=(pwB[1],), writes=(lamB,))
        k.op(dve, lambda: nc.vector.tensor_copy(out=LI2b[:, 1, :], in_=Pim[:, 0:64]), reads=(pwB[1],), writes=(lamB,))
        for fc in range(16):
            k.op(dve, lambda fc=fc: nc.vector.tensor_scalar(out=Dz[:, fc, :], in0=ident[:], scalar1=vecs[:, DSK + fc:DSK + fc + 1], scalar2=None, op0=ALU.mult), reads=(idB, vB), writes=(DzB,))
        k.barrier()

    k.init_slots()
    xT = sb(es, "xT", [128, 32, NT], F32)
    xB = bufs(32)

    def load_x(src_d, t0, N):
        for q in range(4):
            k.dma(sp, xT[:, q * 8:(q + 1) * 8, 0:N], src_d[q * 8:(q + 1) * 8, :, t0:t0 + N].rearrange("c p t -> p c t"),
                  writes=tuple(xB[q * 8:(q + 1) * 8]))

    def rmsnorm(N, goff, emit):
        pt, pb = k.psum()
        for c in range(32):
            s, sB_ = sq[c % 2], sqB[c % 2]
            k.op(act, lambda c=c, s=s: nc.scalar.activation(out=s[:, 0:N], in_=xT[:, c, 0:N], func=AF.Square), reads=(xB[c],), writes=(sB_,))
            k.op(pe, lambda c=c, s=s: nc.tensor.matmul(pt[:, 0:N], lhsT=ones[:], rhs=s[:, 0:N], start=(c == 0), stop=(c == 31)), reads=(onB, sB_), writes=(pb,))
        k.op(act, lambda: nc.scalar.activation(out=rstd[:, 0:N], in_=pt[:, 0:N], func=AF.Sqrt, bias=epsT[:], scale=1.0 / D), reads=(pb, epB), writes=(rsB,))
        k.op(dve, lambda: nc.vector.reciprocal(out=rstd[:, 0:N], in_=rstd[:, 0:N]), reads=(rsB,), writes=(rsB,))
        for c in range(32):
            o_ap, o_buf, post = emit(c)
            k.op(dve, lambda c=c, o_ap=o_ap: nc.vector.scalar_tensor_tensor(out=o_ap, in0=xT[:, c, 0:N], scalar=vecs[:, goff + c:goff + c + 1], in1=rstd[:, 0:N], op0=ALU.mult, op1=ALU.mult),
                 reads=(xB[c], vB, rsB), writes=(o_buf,))
            if post:
                post(c)

    def load_cz():
        k.barrier()
        for h in range(4):
            k.dma(pool, Cz[:, h * 16:(h + 1) * 16, 0, :], cz_d[0][:, h * 16:(h + 1) * 16, :], writes=(CzB,))
            k.dma(pool, Cz[:, h * 16:(h + 1) * 16, 1, :], cz_d[1][:, h * 16:(h + 1) * 16, :], writes=(CzB,))
        k.op(dve, lambda: nc.vector.tensor_scalar(out=Cz[:, :, 1, :], in0=Cz[:, :, 1, :], scalar1=-1.0, scalar2=None, op0=ALU.mult), reads=(CzB,), writes=(CzB,))
        k.barrier()

    def mixer_pre_all():
        N = NT
        HN = 128
        NBK = HN // LB
        BqP = Cz[:].rearrange("p g r m -> p (g r m)").rearrange("p (a f r m) -> p a f r m", a=LB, f=16, r=2, m=128)
        with ExitStack() as ph:
            actA = sb(ph, "actA", [128, 32, NT], BF16)
            aB = bufs(32)
            uT2 = [sb(ph, "uTp%d" % i, [128, 16, NT], BF16) for i in range(2)]
            uB2 = [bufs(16), bufs(16)]
            uTm2 = [sb(ph, "uTmP%d" % i, [128, 4, 16, HN], BF16) for i in range(1)] * 2
            umB2 = bufs(1) * 2
            BUc2 = [sb(ph, "BUc%d" % i, [128, NBK, 2, 64], F32) for i in range(2)]
            BUB2 = bufs(2)
            HS = sb(ph, "HSp", [128, 2, 2, 64], F32)
            HSB = [Buf(), Buf()]
            P1 = sb(ph, "P1p", [128, 2, 64], F32)
            P2 = sb(ph, "P2p", [128, 2, 64], F32)
            PB = [(Buf(), Buf()), (Buf(), Buf())]
            halves = [(dve, nc.vector, 0, 32), (dve, nc.vector, 32, 64)]
            def stage_a1(ist):
                conv_pump(16)
                load_x(xpre_d, ist * NT, N)
                rmsnorm(N, GM, lambda c: (actA[:, c, 0:N], aB[c], None))

            def stage_a2(ist):
                uT, uB = uT2[ist % 2], uB2[ist % 2]

                def evac_in(mc, pt, pb):
                    k.op(act, lambda: nc.scalar.copy(out=uT[:, mc - 16, 0:N], in_=pt[:, 0:N]), reads=(pb,), writes=(uB[mc - 16],))
                k.linear(w_in_h, 32, 4, [4, 5, 6, 7], lambda kc: (actA[:, kc, 0:N], aB[kc]), N, evac_in)

            def stage_b1(ist):
                uT, uB = uT2[ist % 2], uB2[ist % 2]
                for hf in range(N // HN):
                    c0 = hf * HN
                    uTm, umB, BUc, BUB = uTm2[hf % 2], umB2[hf % 2], BUc2[hf % 2], BUB2[hf % 2]
                    for q in range(4):
                        k.op(act, lambda q=q: nc.scalar.activation(out=uTm[:, q, :, :], in_=uT[:, :, c0:c0 + HN], func=AF.Copy, scale=rmask[:, q:q + 1]), reads=tuple(uB) + (rmB,), writes=(umB,))
                    for bk in range(8):
                        pt, pb = k.psum()
                        for gl_ in range(8):
                            gj = bk * 8 + gl_
                            fc, q = gj // 4, gj % 4
                            for ri in range(2):
                                col = (gl_ * 2 + ri) * NBK
                                for a_ in range(LB):
                                    k.op(pe, lambda pt=pt, fc=fc, q=q, ri=ri, col=col, a_=a_: nc.tensor.matmul(
                                        pt[:, col:col + NBK], lhsT=BqP[:, a_, fc, ri, :], rhs=uTm[:, q, fc, :].rearrange("p (j a) -> p a j", a=LB)[:, a_, :],
                                        start=(a_ == 0), stop=(a_ == LB - 1)), reads=(CzB, umB), writes=(pb,))
                        k.op(act, lambda pt=pt, bk=bk: nc.scalar.copy(out=BUc[:, :, :, bk * 8:(bk + 1) * 8], in_=pt[:, 0:512].rearrange("p (g r j) -> p j r g", g=8, r=2, j=NBK)), reads=(pb,), writes=(BUB,))

            def stage_b2(ist):
                for hi, (eng, eo, g0, g1) in enumerate(halves):
                    k.op(eng, lambda eo=eo, g0=g0, g1=g1: eo.tensor_copy(out=HS[:, 0, :, g0:g1], in_=hcar[:, :, g0:g1]), reads=(hcB[hi],), writes=(HSB[hi],))
                cur = 0
                for hf in range(N // HN):
                    BUc, BUB = BUc2[hf % 2], BUB2[hf % 2]
                    for j in range(NBK):
                        nxt = 1 - cur
                        for hi, (eng, eo, g0, g1) in enumerate(halves):
                            p1B, p2B = PB[hi]
                            k.op(eng, lambda eo=eo, g0=g0, g1=g1: eo.tensor_tensor(out=P1[:, :, g0:g1], in0=HS[:, cur, :, g0:g1], in1=LRRb[:, :, g0:g1], op=ALU.mult), reads=(HSB[hi], lamB), writes=(p1B,), skip_self=NOSYNC)
                            k.op(eng, lambda eo=eo, g0=g0, g1=g1: eo.tensor_tensor(out=P2[:, :, g0:g1], in0=HS[:, cur, ::-1, g0:g1], in1=LI2b[:, :, g0:g1], op=ALU.mult), reads=(HSB[hi], lamB), writes=(p2B,), skip_self=NOSYNC)
                        for hi, (eng, eo, g0, g1) in enumerate(halves):
                            p1B, p2B = PB[hi]
                            k.op(eng, lambda eo=eo, g0=g0, g1=g1: eo.tensor_tensor(out=P1[:, :, g0:g1], in0=P1[:, :, g0:g1], in1=P2[:, :, g0:g1], op=ALU.add), reads=(p1B, p2B), writes=(p1B,), skip_self=NOSYNC)
                        for hi, (eng, eo, g0, g1) in enumerate(halves):
                            p1B, p2B = PB[hi]
                            k.op(eng, lambda eo=eo, g0=g0, g1=g1, j=j: eo.tensor_tensor(out=HS[:, nxt, :, g0:g1], in0=P1[:, :, g0:g1], in1=BUc[:, j, :, g0:g1], op=ALU.add), reads=(p1B, BUB), writes=(HSB[hi],), skip_self=NOSYNC)
                        cur = nxt
                for hi, (eng, eo, g0, g1) in enumerate(halves):
                    k.op(eng, lambda eo=eo, g0=g0, g1=g1: eo.tensor_copy(out=hcar[:, :, g0:g1], in_=HS[:, cur, :, g0:g1]), reads=(HSB[hi],), writes=(hcB[hi],))

            nst = PRE // NT
            stage_a1(0)
            stage_a2(0)
            for ist in range(nst):
                if ist + 1 < nst:
                    stage_a1(ist + 1)
                stage_b1(ist)
                if ist + 1 < nst:
                    stage_a2(ist + 1)
                stage_b2(ist)
            conv_pump(10 ** 6)
            k.barrier()

    def mixer(mode, src_d, t0, N, out_idx=None):
        with ExitStack() as ph:
            actA = sb(ph, "actA", [128, 32, NT], BF16)
            aB = bufs(32)
            uT = sb(ph, "uT", [128, 16, NT], BF16)
            uB = bufs(16)
            sT = sb(ph, "sT", [128, 16, NT], BF16)
            sB = bufs(16)
            sg = [sb(ph, "sg%d" % i, [128, NT], F32) for i in range(2)]
            sgB = bufs(2)
            php = ExitStack()
            load_x(src_d, t0, N)
            rmsnorm(N, GM, lambda c: (actA[:, c, 0:N], aB[c], None))
            do_pool = mode in ("halo", "main", "sample")
            do_ssm = mode in ("pre", "main", "sample")
            if do_pool:
                zp = sb(php, "zp", [128, 16, 16 + NT], F32)
                zB = bufs(16)
                tA = sb(php, "tA", [128, 4, 16 + NT], F32)
                tBt = sb(php, "tB", [128, 4, 16 + NT], F32)
                tAB, tBB = Buf(), Buf()
            if mode == "sample":
                segs = [(0, 64, 16), (64, 64, 96)]
                Wd = 160
            else:
                segs = [(0, N, 16)]
                Wd = 16 + N
            if mode in ("main",):
                k.op(act, lambda: nc.scalar.copy(out=zp[:, :, 0:16], in_=halo[:]), reads=(haB,), writes=tuple(zB))
            if mode == "sample":
                for s in range(2):
                    k.dma(sp, zp[:, :, s * 80:s * 80 + 16], cp_d[s], writes=tuple(zB))

            def evac_in(mc, pt, pb):
                if mc < 16:
                    for (ts, tl_, cs) in segs:
                        k.op(act, lambda ts=ts, tl_=tl_, cs=cs: nc.scalar.copy(out=zp[:, mc, cs:cs + tl_], in_=pt[:, ts:ts + tl_]), reads=(pb,), writes=(zB[mc],))
                else:
                    k.op(act, lambda: nc.scalar.copy(out=uT[:, mc - 16, 0:N], in_=pt[:, 0:N]), reads=(pb,), writes=(uB[mc - 16],))

            mgs = []
            if do_pool:
                mgs += [0, 1, 2, 3]
            if do_ssm:
                mgs += [4, 5, 6, 7]
            k.linear(w_in_h, 32, 4, mgs, lambda kc: (actA[:, kc, 0:N], aB[kc]), N, evac_in)

            if mode == "halo":
                k.op(act, lambda: nc.scalar.copy(out=halo[:], in_=zp[:, :, N:N + 16]), reads=tuple(zB), writes=(haB,))
                k.barrier()
                php.close()
                return

            if do_pool:
                for g in range(4):
                    w = 2 << g
                    zg = zp[:, g * 4:(g + 1) * 4, :]
                    zgB = tuple(zB[g * 4:(g + 1) * 4])
                    k.op(dve, lambda zg=zg: nc.vector.tensor_tensor(out=tA[:, :, 1:Wd], in0=zg[:, :, 1:Wd], in1=zg[:, :, 0:Wd - 1], op=ALU.add), reads=zgB, writes=(tAB,))
                    cur, curB, oth, othB = tA, tAB, tBt, tBB
                    off = 1
                    stp = 2
                    while stp < w:
                        no = off + stp
                        k.op(dve, lambda cur=cur, oth=oth, no=no, off=off, stp=stp: nc.vector.tensor_tensor(out=oth[:, :, no:Wd], in0=cur[:, :, no:Wd], in1=cur[:, :, off:Wd - stp], op=ALU.add), reads=(curB,), writes=(othB,))
                        cur, curB, oth, othB = oth, othB, cur, curB
                        off = no
                        stp *= 2
                    if mode == "main" and t0 == 0:
                        k.op(dve, lambda cur=cur, g=g: nc.vector.tensor_tensor(out=cur[:, :, 16:32], in0=cur[:, :, 16:32], in1=rcorr[:, g, :].unsqueeze(1).broadcast_to([128, 4, 16]), op=ALU.mult), reads=(curB, rcB), writes=(curB,))
                    for (ts, tl_, cs) in segs:
                        k.op(dve, lambda cur=cur, zg=zg, ts=ts, tl_=tl_, cs=cs, g=g, w=w: nc.vector.scalar_tensor_tensor(
                            out=actA[:, 16 + g * 4:16 + (g + 1) * 4, ts:ts + tl_], in0=cur[:, :, cs:cs + tl_], scalar=1.0 / w, in1=zg[:, :, cs:cs + tl_], op0=ALU.mult, op1=ALU.subtract),
                            reads=(curB,) + zgB, writes=tuple(aB[16 + g * 4:16 + (g + 1) * 4]))
                if mode == "main":
                    k.op(act, lambda: nc.scalar.copy(out=halo[:], in_=zp[:, :, N:N + 16]), reads=tuple(zB), writes=(haB,))
                    if out_idx is not None:
                        k.dma(sp, pool_o[0], zp[:, :, N:N + 16], reads=tuple(zB))
                if mode == "sample":
                    for s in range(2):
                        k.dma(sp, pool_o[1 + s], zp[:, :, s * 80 + 64:s * 80 + 80], reads=tuple(zB))
                for g in range(4):
                    def evac_p(mc, pt, pb, g=g):
                        c = g * 4 + mc
                        k.op(act, lambda: nc.scalar.activation(out=actA[:, c, 0:N], in_=pt[:, 0:N], func=AF.Copy, scale=vecs[:, PSC + c:PSC + c + 1]), reads=(pb, vB), writes=(aB[c],))
                    k.linear(w_pool_h[g:g + 1], 4, 4, [0], lambda kc, g=g: (actA[:, 16 + g * 4 + kc, 0:N], aB[16 + g * 4 + kc]), N, evac_p)

            k.barrier()
            php.close()
            phs = ExitStack()
            BU2 = [sb(phs, "BU%d" % i, [128, LC, 2, 64], F32) for i in range(2)]
            BUB2 = bufs(2)
            HS2 = [sb(phs, "HS%d" % i, [128, LC + 1, 2, 64], F32) for i in range(2)]
            HSB2 = [[Buf(), Buf()], [Buf(), Buf()]]
            HSb2 = [sb(phs, "HSb%d" % i, [128, LC, 2, 64], BF16) for i in range(2)]
            HbB2 = bufs(2)
            uTm2 = [sb(phs, "uTm%d" % i, [128, 4, 16, LC], BF16) for i in range(2)]
            umB2 = bufs(2)
            P1 = sb(phs, "P1", [128, 2, 64], F32)
            P2 = sb(phs, "P2", [128, 2, 64], F32)
            PB = [(Buf(), Buf()), (Buf(), Buf())]
            halves = [(dve, nc.vector, 0, 32), (dve, nc.vector, 32, 64)]
            nch = N // LC
            for ch in range(nch):
                c0 = ch * LC
                b_ = ch % 2
                BU, BUB, HS, HSB, HSb, HbB, uTm, umB = BU2[b_], BUB2[b_], HS2[b_], HSB2[b_], HSb2[b_], HbB2[b_], uTm2[b_], umB2[b_]
                HSp, HSpB = HS2[1 - b_], HSB2[1 - b_]
                if mode == "sample" and c0 % 64 == 0:
                    s_ = c0 // 64
                    k.dma(sp, HS[:, 0, :, :], st0_d[s_], writes=(HSB[0], HSB[1]))
                elif ch == 0:
                    for hi, (eng, eo, g0, g1) in enumerate(halves):
                        k.op(eng, lambda eo=eo, g0=g0, g1=g1: eo.tensor_copy(out=HS[:, 0, :, g0:g1], in_=hcar[:, :, g0:g1]), reads=(hcB[hi],), writes=(HSB[hi],))
                else:
                    for hi, (eng, eo, g0, g1) in enumerate(halves):
                        k.op(eng, lambda eo=eo, g0=g0, g1=g1: eo.tensor_copy(out=HS[:, 0, :, g0:g1], in_=HSp[:, LC, :, g0:g1]), reads=(HSpB[hi],), writes=(HSB[hi],), skip_self=NOSYNC)
                for q in range(4):
                    k.op(act, lambda q=q: nc.scalar.activation(out=uTm[:, q, :, :], in_=uT[:, :, c0:c0 + LC], func=AF.Copy, scale=rmask[:, q:q + 1]), reads=tuple(uB) + (rmB,), writes=(umB,))
                for bk in range(4):
                    pt, pb = k.psum()
                    for gl_ in range(16):
                        gj = bk * 16 + gl_
                        fc, q = gj // 4, gj % 4
                        for ri in range(2):
                            col = (gl_ * 2 + ri) * LC
                            k.op(pe, lambda pt=pt, fc=fc, q=q, ri=ri, col=col: nc.tensor.matmul(pt[:, col:col + LC], lhsT=Bq[:, fc, ri, :], rhs=uTm[:, q, fc, :], start=True, stop=True),
                                 reads=(BqB, umB), writes=(pb,))
                    k.op(act, lambda pt=pt, bk=bk: nc.scalar.copy(out=BU[:, :, :, bk * 16:(bk + 1) * 16], in_=pt[:, 0:512].rearrange("p (g r t) -> p t r g", g=16, r=2, t=LC)), reads=(pb,), writes=(BUB,))
                for t in range(LC):
                    for hi, (eng, eo, g0, g1) in enumerate(halves):
                        p1B, p2B = PB[hi]
                        k.op(eng, lambda eo=eo, g0=g0, g1=g1, t=t: eo.tensor_tensor(out=P1[:, :, g0:g1], in0=HS[:, t, :, g0:g1], in1=LRR[:, :, g0:g1], op=ALU.mult), reads=(HSB[hi], lamB), writes=(p1B,), skip_self=NOSYNC)
                        k.op(eng, lambda eo=eo, g0=g0, g1=g1, t=t: eo.tensor_tensor(out=P2[:, :, g0:g1], in0=HS[:, t, ::-1, g0:g1], in1=LI2[:, :, g0:g1], op=ALU.mult), reads=(HSB[hi], lamB), writes=(p2B,), skip_self=NOSYNC)
                    for hi, (eng, eo, g0, g1) in enumerate(halves):
                        p1B, p2B = PB[hi]
                        k.op(eng, lambda eo=eo, g0=g0, g1=g1, t=t: eo.tensor_tensor(out=P1[:, :, g0:g1], in0=P1[:, :, g0:g1], in1=P2[:, :, g0:g1], op=ALU.add), reads=(p1B, p2B), writes=(p1B,), skip_self=NOSYNC)
                    for hi, (eng, eo, g0, g1) in enumerate(halves):
                        p1B, p2B = PB[hi]
                        k.op(eng, lambda eo=eo, g0=g0, g1=g1, t=t: eo.tensor_tensor(out=HS[:, t + 1, :, g0:g1], in0=P1[:, :, g0:g1], in1=BU[:, t, :, g0:g1], op=ALU.add), reads=(p1B, BUB), writes=(HSB[hi],), skip_self=NOSYNC)
                k.op(act, lambda: nc.scalar.copy(out=HSb[:], in_=HS[:, 1:LC + 1, :, :]), reads=(HSB[0], HSB[1]), writes=(HbB,))
                pt, pb = k.psum()
                for fc in range(16):
                    ia = 0
                    for q in range(4):
                        gj = fc * 4 + q
                        for ri in range(2):
                            k.op(pe, lambda pt=pt, fc=fc, gj=gj, ri=ri, ia=ia: nc.tensor.matmul(pt[:, fc * LC:(fc + 1) * LC], lhsT=Cz[:, gj, ri, :], rhs=HSb[:, :, ri, gj], start=(ia == 0), stop=False),
                                 reads=(CzB, HbB), writes=(pb,))
                            ia += 1
                    k.op(pe, lambda pt=pt, fc=fc: nc.tensor.matmul(pt[:, fc * LC:(fc + 1) * LC], lhsT=Dz[:, fc, :], rhs=uT[:, fc, c0:c0 + LC], start=False, stop=True), reads=(DzB, uB[fc]), writes=(pb,))
                k.op(act, lambda pt=pt: nc.scalar.activation(out=sT[:, :, c0:c0 + LC], in_=pt[:, 0:16 * LC].rearrange("p (f t) -> p f t", f=16, t=LC), func=AF.Gelu), reads=(pb,), writes=tuple(sB))
                if mode == "sample" and (c0 + LC) % 64 == 0:
                    s_ = c0 // 64
                    k.dma(sp, ssm_o[1 + s_], HS[:, LC, :, :], reads=(HSB[0], HSB[1]))
                if ch == nch - 1 and mode == "main":
                    for hi, (eng, eo, g0, g1) in enumerate(halves):
                        k.op(eng, lambda eo=eo, g0=g0, g1=g1: eo.tensor_copy(out=hcar[:, :, g0:g1], in_=HS[:, LC, :, g0:g1]), reads=(HSB[hi],), writes=(hcB[hi],))
                    if out_idx is not None:
                        k.dma(sp, ssm_o[0], HS[:, LC, :, :], reads=(HSB[0], HSB[1]))

            def evac_glu(mc, pt, pb):
                s_, sB_ = sg[mc % 2], sgB[mc % 2]
                k.op(act, lambda: nc.scalar.activation(out=s_[:, 0:N], in_=pt[:, 0:N], func=AF.Sigmoid, bias=vecs[:, BGL + mc:BGL + mc + 1]), reads=(pb, vB), writes=(sB_,))
                k.op(dve, lambda: nc.vector.tensor_tensor(out=actA[:, 16 + mc, 0:N], in0=sT[:, mc, 0:N], in1=s_[:, 0:N], op=ALU.mult), reads=(sB[mc], sB_), writes=(aB[16 + mc],))
            k.linear(w_glu_h, 16, 4, [0, 1, 2, 3], lambda kc: (sT[:, kc, 0:N], sB[kc]), N, evac_glu)

            def evac_out(mc, pt, pb):
                k.op(dve, lambda: nc.vector.tensor_tensor(out=xT[:, mc, 0:N], in0=pt[:, 0:N], in1=xT[:, mc, 0:N], op=ALU.add), reads=(pb, xB[mc]), writes=(xB[mc],))
            k.linear(w_out_h, 32, 4, list(range(8)), lambda kc: (actA[:, kc, 0:N], aB[kc]), N, evac_out)
            k.barrier()
            phs.close()

    def peer(N):
        ntt = N // 128
        with ExitStack() as ph:
            xn = sb(ph, "xn", [128, 32, NT], BF16)
            xnB = bufs(32)
            eT = [sb(ph, "eT%d" % i, [128, 16, 128], F32) for i in range(ntt)]
            eB = bufs(ntt)
            Dh = [sb(ph, "Dh%d" % i, [128, 8, 128], BF16) for i in range(ntt)]
            DhB = bufs(ntt)
            th = [sb(ph, "th%d" % i, [128, 8], F32) for i in range(ntt)]
            thB = bufs(ntt)
            rmsnorm(N, GF, lambda c: (xn[:, c, 0:N], xnB[c], None))
            with ExitStack() as ph2:
                qT = sb(ph2, "qT", [128, 16, NT], BF16)
                qB = bufs(16)
                keys = sb(ph2, "keys", [128, 16, 128], BF16)
                kyB = Buf()
                k.dma(pool, keys[:], keys_d[:, :, :], writes=(kyB,))

                def evac_q(mc, pt, pb):
                    k.op(act, lambda: nc.scalar.copy(out=qT[:, mc, 0:N], in_=pt[:, 0:N]), reads=(pb,), writes=(qB[mc],))
                k.linear(w_q_h, 32, 4, [0, 1, 2, 3], lambda kc: (xn[:, kc, 0:N], xnB[kc]), N, evac_q)
                mx = sb(ph2, "mx", [128, 16], F32)
                mxB = Buf()
                T16 = sb(ph2, "T16", [128, 16, 16], F32)
                T16B = Buf()
                tmpe = sb(ph2, "tmpe", [128, 128], F32)
                tmpeB = Buf()
                cand = sb(ph2, "cand", [128, 8, 256], F32)
                cdB = Buf()
                tmpc = sb(ph2, "tmpc", [128, 256], F32)
                tcB = Buf()
                C16 = sb(ph2, "C16", [128, 8, 16], F32)
                C16B = Buf()
                Zt = sb(ph2, "Zt", [128, 8], F32)
                ZB = Buf()
                for tt in range(ntt):
                    pss = [k.psum() for _ in range(4)]
                    for hp in range(16):
                        pt, pb = pss[hp // 4]
                        k.op(pe, lambda pt=pt, hp=hp: nc.tensor.matmul(pt[:, (hp % 4) * 128:(hp % 4 + 1) * 128], lhsT=qT[:, hp, tt * 128:(tt + 1) * 128], rhs=keys[:, hp, :], start=True, stop=True),
                             reads=(qB[hp], kyB), writes=(pb,))
                    for b in range(4):
                        pt, pb = pss[b]
                        k.op(dve, lambda pt=pt, b=b: nc.vector.tensor_reduce(out=mx[:, b * 4:(b + 1) * 4], in_=pt[:, 0:512].rearrange("p (a n) -> p a n", a=4, n=128), axis=AX.X, op=ALU.max), reads=(pb,), writes=(mxB,))
                    k.op(dve, lambda: nc.vector.tensor_scalar(out=mx[:], in0=mx[:], scalar1=-1.0, scalar2=None, op0=ALU.mult), reads=(mxB,), writes=(mxB,))
                    for hp in range(16):
                        pt, pb = pss[hp // 4]
                        k.op(act, lambda pt=pt, hp=hp: nc.scalar.activation(out=eT[tt][:, hp, :], in_=pt[:, (hp % 4) * 128:(hp % 4 + 1) * 128], func=AF.Exp, bias=mx[:, hp:hp + 1]), reads=(pb, mxB), writes=(eB[tt],))
                    for hp in range(16):
                        k.op(dve, lambda hp=hp: nc.vector.max(out=T16[:, hp, 0:8], in_=eT[tt][:, hp, :]), reads=(eB[tt],), writes=(T16B,))
                        k.op(dve, lambda hp=hp: nc.vector.match_replace(out=tmpe[:], in_to_replace=T16[:, hp, 0:8], in_values=eT[tt][:, hp, :], imm_value=-1.0), reads=(eB[tt], T16B), writes=(tmpeB,))
                        k.op(dve, lambda hp=hp: nc.vector.max(out=T16[:, hp, 8:16], in_=tmpe[:]), reads=(tmpeB,), writes=(T16B,))
                    for h in range(8):
                        k.op(dve, lambda h=h: nc.vector.tensor_tensor(out=cand[:, h, :].rearrange("p (a b) -> p a b", a=16, b=16), in0=T16[:, 2 * h, :].unsqueeze(2).broadcast_to([128, 16, 16]),
                                                                      in1=T16[:, 2 * h + 1, :].unsqueeze(1).broadcast_to([128, 16, 16]), op=ALU.mult), reads=(T16B,), writes=(cdB,))
                        k.op(dve, lambda h=h: nc.vector.max(out=C16[:, h, 0:8], in_=cand[:, h, :]), reads=(cdB,), writes=(C16B,))
                        k.op(dve, lambda h=h: nc.vector.match_replace(out=tmpc[:], in_to_replace=C16[:, h, 0:8], in_values=cand[:, h, :], imm_value=-1.0), reads=(cdB, C16B), writes=(tcB,))
                        k.op(dve, lambda h=h: nc.vector.max(out=C16[:, h, 8:16], in_=tmpc[:]), reads=(tcB,), writes=(C16B,))
                    k.op(dve, lambda: nc.vector.tensor_reduce(out=Zt[:], in_=C16[:], axis=AX.X, op=ALU.add), reads=(C16B,), writes=(ZB,))
                    k.op(dve, lambda: nc.vector.reciprocal(out=Zt[:], in_=Zt[:]), reads=(ZB,), writes=(ZB,))
                    k.op(dve, lambda: nc.vector.tensor_copy(out=th[tt][:], in_=C16[:, :, 15]), reads=(C16B,), writes=(thB[tt],))
                    for h in range(8):
                        k.op(dve, lambda h=h: nc.vector.tensor_scalar(out=Dh[tt][:, h, :], in0=ident[:], scalar1=Zt[:, h:h + 1], scalar2=None, op0=ALU.mult), reads=(idB, ZB), writes=(DhB[tt],))
                k.barrier()

            NG = 32
            Gh = [[sb(ph, "Gh%d_%d" % (p_, i), [128, 8, 4, 128], BF16) for i in range(ntt)] for p_ in range(2)]
            GhB = [bufs(ntt) for _ in range(2)]
            Eb = [sb(ph, "Eb%d" % i, [128, 4, 128], F32) for i in range(2)]
            EbB = bufs(2)
            gl = sb(ph, "gl", [128, 2, 4, NT], BF16)
            glB = [bufs(4), bufs(4)]
            AT = sb(ph, "AT", [128, 2, 4, NT], BF16)
            ATB = [bufs(4), bufs(4)]
            cnt = {"ei": 0}

            def emit_G(eg, pieces=None):
                pp = eg % 2
                for tt in range(ntt):
                    for h in range(8):
                        if pieces is not None and (tt * 8 + h) not in pieces:
                            continue
                        E_, EB_ = Eb[cnt["ei"] % 2], EbB[cnt["ei"] % 2]
                        cnt["ei"] += 1
                        k.op(pool, lambda E_=E_, tt=tt, h=h: nc.gpsimd.tensor_tensor(out=E_[:], in0=eT[tt][:, 2 * h, eg * 4:(eg + 1) * 4].unsqueeze(2).broadcast_to([128, 4, 128]),
                                                                                   in1=eT[tt][:, 2 * h + 1, :].unsqueeze(1).broadcast_to([128, 4, 128]), op=ALU.mult), reads=(eB[tt],), writes=(EB_,))
                        k.op(dve, lambda E_=E_, tt=tt, h=h: nc.vector.scalar_tensor_tensor(out=Gh[pp][tt][:, h, :, :].rearrange("p a b -> p (a b)"), in0=E_[:].rearrange("p a b -> p (a b)"), scalar=th[tt][:, h:h + 1],
                                                                                         in1=E_[:].rearrange("p a b -> p (a b)"), op0=ALU.is_ge, op1=ALU.mult), reads=(EB_, thB[tt]), writes=(GhB[pp][tt],))

            items = []
            stt = {}
            for eg in range(NG):
                pp = eg % 2
                for j in range(4):
                    ec = eg * 4 + j
                    for half in range(2):
                        def cons_u(wt, wb, eg=eg, pp=pp, j=j, half=half):
                            if eg == 0 and j == 0 and half == 0:
                                emit_G(0)
                            if half == 0:
                                stt["ph"] = k.psum()
                            pt, pb = stt["ph"]
                            for dcl in range(16):
                                dc = half * 16 + dcl
                                k.op(pe, lambda pt=pt, dcl=dcl, dc=dc: nc.tensor.matmul(pt[:, 0:N], lhsT=wt[:, dcl * 128:(dcl + 1) * 128], rhs=xn[:, dc, 0:N], start=(dc == 0), stop=(dc == 31)),
                                     reads=(wb, xnB[dc]), writes=(pb,))
                            if half == 1:
                                k.op(act, lambda pt=pt: nc.scalar.activation(out=gl[:, pp, j, 0:N], in_=pt[:, 0:N], func=AF.Gelu), reads=(pb,), writes=(glB[pp][j],))
                                pg, pgb = k.psum()
                                for tt in range(ntt):
                                    for h in range(8):
                                        k.op(pe, lambda pg=pg, tt=tt, h=h: nc.tensor.matmul(pg[:, tt * 128:(tt + 1) * 128], lhsT=Gh[pp][tt][:, h, j, :], rhs=Dh[tt][:, h, :], start=(h == 0), stop=(h == 7)),
                                             reads=(GhB[pp][tt], DhB[tt]), writes=(pgb,))
                                k.op(dve, lambda pg=pg: nc.vector.tensor_tensor(out=AT[:, pp, j, 0:N], in0=pg[:, 0:N], in1=gl[:, pp, j, 0:N], op=ALU.mult), reads=(pgb, glB[pp][j]), writes=(ATB[pp][j],))
                        items.append((ut_h[ec, half], (16, 128), cons_u))
                for half in range(2):
                    for j in range(4):
                        ec = eg * 4 + j

                        def cons_v(wt, wb, eg=eg, pp=pp, j=j, half=half):
                            if j == 0:
                                stt["v"] = []
                            stt["v"].append((wt, wb))
                            if j == 3:
                                vs = stt["v"]
                                for dcl in range(16):
                                    dc = half * 16 + dcl
                                    pt, pb = k.psum()
                                    for jj in range(4):
                                        vt, vb = vs[jj]
                                        k.op(pe, lambda pt=pt, vt=vt, jj=jj, dcl=dcl: nc.tensor.matmul(pt[:, 0:N], lhsT=vt[:, dcl * 128:(dcl + 1) * 128], rhs=AT[:, pp, jj, 0:N], start=(jj == 0), stop=(jj == 3)),
                                             reads=(vb, ATB[pp][jj]), writes=(pb,))
                                    k.op(dve, lambda pt=pt, dc=dc: nc.vector.tensor_tensor(out=xT[:, dc, 0:N], in0=pt[:, 0:N], in1=xT[:, dc, 0:N], op=ALU.add), reads=(pb, xB[dc]), writes=(xB[dc],))
                                    if eg + 1 < NG:
                                        npc = ntt * 8
                                        lo = (dc * npc) // 32
                                        hi_ = ((dc + 1) * npc) // 32
                                        if hi_ > lo:
                                            emit_G(eg + 1, set(range(lo, hi_)))
                        items.append((v_h[ec][:, half * 2048:(half + 1) * 2048].rearrange("p (a b) -> p a b", a=4, b=512), (4, 512), cons_v))
            k.stream(items, LA=4)
            k.barrier()

    def ple_final(t0, N):
        with ExitStack() as ph:
            xn = sb(ph, "xn2", [128, 32, NT], BF16)
            xnB = bufs(32)
            gate = sb(ph, "gate", [128, 32, NT], BF16)
            gB = bufs(32)
            pTt = sb(ph, "pTt", [128, 2, NT], BF16)
            pB = Buf()
            tmp = [sb(ph, "ptmp%d" % i, [128, NT], F32) for i in range(2)]
            tmB = bufs(2)
            yo = [sb(ph, "yo%d" % i, [128, NT], F32) for i in range(4)]
            yoB = bufs(4)
            k.dma(pool, pTt[:, :, 0:N], pT_d[:, :, t0:t0 + N].rearrange("c p t -> p c t"), writes=(pB,))
            rmsnorm(N, GP, lambda c: (xn[:, c, 0:N], xnB[c], None))

            def evac_g(mc, pt, pb):
                k.op(act, lambda: nc.scalar.activation(out=gate[:, mc, 0:N], in_=pt[:, 0:N], func=AF.Sigmoid), reads=(pb,), writes=(gB[mc],))
            k.linear(w_pg_h, 32, 4, list(range(8)), lambda kc: (xn[:, kc, 0:N], xnB[kc]), N, evac_g)

            def evac_p(mc, pt, pb):
                t_, tB_ = tmp[mc % 2], tmB[mc % 2]
                k.op(dve, lambda: nc.vector.tensor_tensor(out=t_[:, 0:N], in0=pt[:, 0:N], in1=gate[:, mc, 0:N], op=ALU.mult), reads=(pb, gB[mc]), writes=(tB_,))
                k.op(dve, lambda: nc.vector.tensor_tensor(out=xT[:, mc, 0:N], in0=t_[:, 0:N], in1=xT[:, mc, 0:N], op=ALU.add), reads=(tB_, xB[mc]), writes=(xB[mc],))
            k.linear(w_ple_h, 2, 2, list(range(8)), lambda kc: (pTt[:, kc, 0:N], pB), N, evac_p)

            def emit(c):
                def post(c):
                    k.dma(sp, yT_d[c, :, t0:t0 + N], yo[c % 4][:, 0:N], reads=(yoB[c % 4],))
                return yo[c % 4][:, 0:N], yoB[c % 4], post
            rmsnorm(N, GL, emit)
            k.barrier()

    k.barrier()
    conv_pump(32)
    k.barrier()
    mixer_pre_all()
    load_cz()
    mixer("halo", xpre_d, PRE - 128, 128)
    nmain = SEG // NT
    for i in range(nmain):
        mixer("main", xT_d, i * NT, NT, out_idx=(0 if i == nmain - 1 else None))
        peer(NT)
        ple_final(i * NT, NT)
    mixer("sample", xT_d, SEG, NSAMP)
    peer(NSAMP)
    ple_final(SEG, NSAMP)
    for so in k.allsems:
        v = so.total
        if v > 0:
            sp.obj.wait_ge(so.h, v)
    k.barrier()
    es.close()
    return nc


def _fm(a):
    T, Fd = a.shape
    return np.ascontiguousarray(a.T.reshape(Fd // 128, 128, T))


def _blk(W, KBk=4):
    K, M = W.shape
    KC = K // 128
    KG = KC // KBk
    MG = M // 512
    return np.ascontiguousarray(W.reshape(KG, KBk, 128, MG, 512).transpose(3, 0, 2, 1, 4))


_NC_CACHE = {}


def kernel(x_prompt, x_sample, p_prompt, p_sample, cache_pool, state_ssm_re, state_ssm_im,
           g_mix, w_in, w_pool, pool_scale, ssm_a_re, ssm_a_im, ssm_log_dt, ssm_b_re, ssm_b_im,
           ssm_c_re, ssm_c_im, ssm_d, w_glu, b_glu, w_out, g_ffn, w_query, peer_sub_keys,
           expert_u, expert_v, g_ple, w_ple_gate, w_ple, g_final):
    f = np.float32
    A = lambda a: np.asarray(a, dtype=f)
    x_prompt, x_sample, p_prompt, p_sample = A(x_prompt), A(x_sample), A(p_prompt), A(p_sample)
    cache_pool, state_ssm_re, state_ssm_im = A(cache_pool), A(state_ssm_re), A(state_ssm_im)
    shared = {}
    vecs = np.zeros((128, 176), f)
    for off, g in ((0, g_mix), (32, g_ffn), (64, g_ple), (96, g_final)):
        vecs[:, off:off + 32] = A(g).reshape(32, 128).T
    vecs[:, 128:144] = A(pool_scale).reshape(16, 128).T
    vecs[:, 144:160] = A(b_glu).reshape(16, 128).T
    vecs[:, 160:176] = A(ssm_d).reshape(16, 128).T
    shared["vecs"] = vecs
    shared["ident"] = np.eye(128, dtype=f)
    rmask = np.zeros((128, 4), f)
    for q in range(4):
        rmask[q * 32:(q + 1) * 32, q] = 1.0
    shared["rmask"] = rmask
    are, aim, ldt = A(ssm_a_re)[0], A(ssm_a_im)[0], A(ssm_log_dt)[0]
    ldt_full = np.repeat(ldt[:, None], 64, axis=1)
    lamp = np.zeros((128, 3, NLAM), f)
    for i, a in enumerate((are, aim, ldt_full)):
        lamp[:, i, 0:64] = a.reshape(64, 2, 64).transpose(1, 2, 0).reshape(128, 64)
        a4 = a.reshape(16, 4, 2, 64)
        l2 = a4.transpose(1, 0, 2, 3).reshape(4, 16 * 128)
        lamp[:, i, 64:] = np.repeat(l2, 32, axis=0)
    shared["lamp"] = lamp
    bq = np.zeros((2, 128, 16, 128), f)
    for ri, b in enumerate((A(ssm_b_re)[0], A(ssm_b_im)[0])):
        b6 = b.reshape(16, 4, 2, 64, 16)
        for e in range(2):
            blk = b6[:, :, e].transpose(1, 3, 0, 2)
            bq[ri].reshape(4, 2, 16, 16, 2, 64)[:, e, :, :, e, :] = blk
    shared["bq"] = bq
    cz = np.zeros((2, 128, 64, 128), f)
    for ri, cc in enumerate((A(ssm_c_re)[0], A(ssm_c_im)[0])):
        c5 = cc.reshape(64, 2, 16, 64)
        czv = cz[ri].reshape(2, 64, 64, 8, 16)
        for gj in range(64):
            for e in range(2):
                czv[e, :, gj, (2 * gj + e) % 8, :] = c5[gj, e].T
    shared["cz"] = cz
    shared["keysT"] = np.ascontiguousarray(A(peer_sub_keys)[0].reshape(16, 128, 128).transpose(2, 0, 1))
    shared["w_in_b"] = _blk(A(w_in)[0])
    shared["w_pool_b"] = np.stack([_blk(A(w_pool)[0, g])[0] for g in range(4)])
    shared["w_glu_b"] = _blk(A(w_glu)[0])
    shared["w_out_b"] = _blk(A(w_out)[0])
    shared["w_q_b"] = _blk(A(w_query)[0])
    shared["w_pg_b"] = _blk(A(w_ple_gate)[0])
    shared["w_ple_b"] = _blk(A(w_ple)[0], 2)
    shared["UTb"] = np.ascontiguousarray(A(expert_u)[0].reshape(128, 128, 2, 16, 128).transpose(0, 2, 4, 3, 1))
    shared["Vn"] = np.ascontiguousarray(A(expert_v)[0].reshape(128, 128, 4096))

    in_maps = []
    for c in range(NCORES):
        b, kq = c // 4, c % 4
        m = dict(shared)
        xs = np.concatenate([x_prompt[b, kq * SEG:(kq + 1) * SEG], x_sample[2 * c], x_sample[2 * c + 1]], axis=0)
        m["xT"] = _fm(xs)
        xp = np.zeros((PRE, D), f)
        if kq > 0:
            xp[PRE - kq * SEG:] = x_prompt[b, 0:kq * SEG]
        m["xpreT"] = _fm(xp)
        ps_ = np.concatenate([p_prompt[0, b, kq * SEG:(kq + 1) * SEG], p_sample[0, 2 * c], p_sample[0, 2 * c + 1]], axis=0)
        m["pT"] = _fm(ps_)
        cp = np.zeros((2, 128, 16, 16), f)
        st0 = np.zeros((2, 128, 2, 64), f)
        for s in range(2):
            cpad = np.concatenate([np.zeros((1, 2048), f), cache_pool[0, 2 * c + s]], axis=0)
            cp[s] = cpad.T.reshape(16, 128, 16).transpose(1, 0, 2)
            for ri, stt in enumerate((state_ssm_re, state_ssm_im)):
                st0[s, :, ri, :] = stt[0, 2 * c + s].reshape(64, 2, 64).transpose(1, 2, 0).reshape(128, 64)
        m["cpT"] = cp
        m["st0"] = st0
        rc = np.ones((128, 4, 16), f)
        for g in range(4):
            w = 2 << g
            for t in range(16):
                pos = kq * SEG + t
                rc[:, g, t] = w / min(pos + 1, w)
        m["rcorr"] = rc
        in_maps.append(m)

    if "nc" not in _NC_CACHE:
        _NC_CACHE["nc"] = build_program()
    nc = _NC_CACHE["nc"]
    res = run_bass_kernel_spmd(nc, in_maps, core_ids=list(range(NCORES)))
    R = res.results

    def unfm(yT):
        return yT.reshape(4096, -1).T

    y_prompt = np.zeros((2, 8192, D), f)
    y_sample = np.zeros((16, 64, D), f)
    new_pool_p = np.zeros((1, 2, 15, 2048), f)
    new_re_p = np.zeros((1, 2, 128, 64), f)
    new_im_p = np.zeros((1, 2, 128, 64), f)
    new_pool_s = np.zeros((1, 16, 15, 2048), f)
    new_re_s = np.zeros((1, 16, 128, 64), f)
    new_im_s = np.zeros((1, 16, 128, 64), f)

    def unpool(po):
        return po.transpose(1, 0, 2).reshape(2048, 16).T[1:]

    def unst(so):
        return so.reshape(2, 64, 64).transpose(2, 0, 1).reshape(128, 64)

    for c in range(NCORES):
        b, kq = c // 4, c % 4
        y = unfm(R[c]["yT"])
        y_prompt[b, kq * SEG:(kq + 1) * SEG] = y[0:SEG]
        y_sample[2 * c] = y[SEG:SEG + 64]
        y_sample[2 * c + 1] = y[SEG + 64:SEG + 128]
        po, so = R[c]["pool_o"], R[c]["ssm_o"]
        if kq == 3:
            new_pool_p[0, b] = unpool(po[0])
            new_re_p[0, b] = unst(so[0][:, 0, :])
            new_im_p[0, b] = unst(so[0][:, 1, :])
        for s in range(2):
            new_pool_s[0, 2 * c + s] = unpool(po[1 + s])
            new_re_s[0, 2 * c + s] = unst(so[1 + s][:, 0, :])
            new_im_s[0, 2 * c + s] = unst(so[1 + s][:, 1, :])
    return (y_prompt, y_sample, new_pool_p, new_re_p, new_im_p, new_pool_s, new_re_s, new_im_s)
```

```python
import math
import numpy as np
from contextlib import ExitStack
import concourse.bass as bass
import concourse.mybir as mybir
from concourse.bass_utils import run_bass_kernel_spmd

F32 = mybir.dt.float32
BF16 = mybir.dt.bfloat16
I32 = mybir.dt.int32
AF = mybir.ActivationFunctionType
ALU = mybir.AluOpType
AX = mybir.AxisListType

NCORES = 8
D = 4096
SEG = 2048
NSAMP = 128
NTOK = SEG + NSAMP
NT = 256
PRE = 3 * SEG
LC = 16
LB = 4
REV_AP = True
EPS = 1e-6
NLAM = 64 + 2048
TWO_PI = 2.0 * math.pi


class SemObj:
    def __init__(self, h):
        self.h = h
        self.total = 0


class Eng:
    def __init__(self, k, name, obj):
        self.name = name
        self.obj = obj
        self.sem = SemObj(k.es.enter_context(k.nc.semaphore("s_" + name)))
        self.count = 0
        self.seen = {}


class Buf:
    __slots__ = ("w", "r")

    def __init__(self):
        self.w = None
        self.r = {}


def bufs(n):
    return [Buf() for _ in range(n)]


class KB:
    def __init__(self):
        self.nc = bass.Bass("TRN2", target_bir_lowering=False)
        self.es = ExitStack()
        nc = self.nc
        self.pe = Eng(self, "pe", nc.tensor)
        self.act = Eng(self, "act", nc.scalar)
        self.dve = Eng(self, "dve", nc.vector)
        self.pool = Eng(self, "pool", nc.gpsimd)
        self.sp = Eng(self, "sp", nc.sync)
        self.engs = [self.pe, self.act, self.dve, self.pool, self.sp]
        self.gsems = [SemObj(self.es.enter_context(nc.semaphore("g%d" % i))) for i in range(8)]
        self.gi = 0
        self.allsems = [e.sem for e in self.engs] + list(self.gsems)
        self.ps = []
        for i in range(8):
            t = self.es.enter_context(nc.psum_tensor("ps%d" % i, [128, 512], F32))
            self.ps.append((t, Buf()))
        self.pi = 0
        self.NS = 8
        self.wslots = []
        self.wi = 0
        self.csems = [SemObj(self.es.enter_context(nc.semaphore("c%d" % i))) for i in range(16)]
        self.allsems += self.csems
        self.ci = 0

    def init_slots(self):
        nc = self.nc
        for i in range(self.NS):
            t = self.es.enter_context(nc.sbuf_tensor("wsl%d" % i, [128, 2048], BF16))
            so = SemObj(self.es.enter_context(nc.semaphore("w%d" % i)))
            self.allsems.append(so)
            self.wslots.append((t, Buf(), so))

    def _waits(self, eng, reads, writes, extra=None):
        deps = {}

        def add(so, v):
            if deps.get(so, 0) < v:
                deps[so] = v

        for b in reads:
            if b.w is not None:
                add(*b.w)
        for b in writes:
            if b.w is not None:
                add(*b.w)
            for so, v in b.r.items():
                add(so, v)
        if extra:
            for so, v in extra:
                add(so, v)
        for so, v in deps.items():
            if so is eng.sem and eng is self.pe:
                continue
            if eng.seen.get(so, 0) >= v:
                continue
            eng.obj.wait_ge(so.h, v)
            eng.seen[so] = v

    def _mark(self, d, reads, writes):
        so, v = d
        for b in reads:
            if b.r.get(so, 0) < v:
                b.r[so] = v
        for b in writes:
            b.w = d
            b.r = {}

    def op(self, eng, fn, reads=(), writes=()):
        self._waits(eng, reads, writes)
        ins = fn()
        eng.count += 1
        ins.then_inc(eng.sem.h, 1)
        self._mark((eng.sem, eng.count), reads, writes)

    def dma(self, q, out, in_, reads=(), writes=(), so=None):
        if so is None:
            so = self.gsems[self.gi % len(self.gsems)]
            self.gi += 1
        extra = [(so, so.total)] if so.total > 0 else None
        self._waits(q, reads, writes, extra)
        ins = q.obj.dma_start(out=out, in_=in_)
        so.total += 16
        ins.then_inc(so.h, 16)
        self._mark((so, so.total), reads, writes)

    def barrier(self):
        for e in self.engs:
            for so in self.allsems:
                v = so.total if so.total > 0 else 0
                for f in self.engs:
                    if f.sem is so:
                        v = f.count
                if v > 0 and e.seen.get(so, 0) < v and not (so is e.sem):
                    e.obj.wait_ge(so.h, v)
                    e.seen[so] = v

    def psum(self):
        r = self.ps[self.pi % 8]
        self.pi += 1
        return r

    def sb(self, stack, name, shape, dt):
        self.uid = getattr(self, "uid", 0) + 1
        return stack.enter_context(self.nc.sbuf_tensor("sb_%s_%d" % (name, self.uid), shape, dt))

    def wload(self, src_ap, n3):
        t, b, so = self.wslots[self.wi % self.NS]
        self.wi += 1
        a, bb = n3
        dst = t[:, 0:a * bb].rearrange("p (a b) -> p a b", a=a, b=bb)
        q = self.sp if src_ap.dtype == BF16 else self.pool
        self.dma(q, dst, src_ap, reads=(), writes=(b,), so=so)
        return t, b

    def stream(self, items, LA=4):
        n = len(items)
        loaded = []
        for i in range(min(LA, n)):
            loaded.append(self.wload(items[i][0], items[i][1]))
        for i in range(n):
            wt, wb = loaded[i]
            items[i][2](wt, wb)
            if i + LA < n:
                loaded.append(self.wload(items[i + LA][0], items[i + LA][1]))

    def linear(self, wd, KC, KBk, mgs, act_in, N, evac):
        nc = self.nc
        KG = KC // KBk
        items = []
        st = {}
        for mg in mgs:
            for kg in range(KG):
                def consume(wt, wb, mg=mg, kg=kg):
                    if kg == 0:
                        st["ps"] = [self.psum() for _ in range(4)]
                    for kb in range(KBk):
                        kc = kg * KBk + kb
                        a_ap, a_buf = act_in(kc)
                        for mi in range(4):
                            pt, pb = st["ps"][mi]
                            self.op(self.pe, lambda pt=pt, kb=kb, mi=mi, a_ap=a_ap, kc=kc: nc.tensor.matmul(
                                pt[:, 0:N], lhsT=wt[:, kb * 512 + mi * 128: kb * 512 + (mi + 1) * 128], rhs=a_ap,
                                start=(kc == 0), stop=(kc == KC - 1)), reads=(wb, a_buf), writes=(pb,))
                    if kg == KG - 1:
                        for mi in range(4):
                            pt, pb = st["ps"][mi]
                            evac(mg * 4 + mi, pt, pb)
                items.append((wd[mg, kg], (KBk, 512), consume))
        self.stream(items, LA=6)


def build_program():
    k = KB()
    nc = k.nc
    es = k.es
    pe, act, dve, pool, sp = k.pe, k.act, k.dve, k.pool, k.sp

    def din(name, shape):
        return nc.dram_tensor(name, list(shape), F32, kind="ExternalInput").ap()

    def dout(name, shape):
        return nc.dram_tensor(name, list(shape), F32, kind="ExternalOutput").ap()

    xT_d = din("xT", [32, 128, NTOK])
    xpre_d = din("xpreT", [32, 128, PRE])
    pT_d = din("pT", [2, 128, NTOK])
    cp_d = din("cpT", [2, 128, 16, 16])
    st0_d = din("st0", [2, 128, 2, 64])
    rcorr_d = din("rcorr", [128, 4, 16])
    vecs_d = din("vecs", [128, 176])
    ident_d = din("ident", [128, 128])
    rmask_d = din("rmask", [128, 4])
    lamp_d = din("lamp", [128, 3, NLAM])
    bq_d = din("bq", [2, 128, 16, 128])
    cz_d = din("cz", [2, 128, 64, 128])
    keys_d = din("keysT", [128, 16, 128])
    w_in_d = din("w_in_b", [8, 8, 128, 4, 512])
    w_pool_d = din("w_pool_b", [4, 1, 128, 4, 512])
    w_glu_d = din("w_glu_b", [4, 4, 128, 4, 512])
    w_out_d = din("w_out_b", [8, 8, 128, 4, 512])
    w_q_d = din("w_q_b", [4, 8, 128, 4, 512])
    w_pg_d = din("w_pg_b", [8, 8, 128, 4, 512])
    w_ple_d = din("w_ple_b", [8, 1, 128, 2, 512])
    ut_d = din("UTb", [128, 2, 128, 16, 128])
    v_d = din("Vn", [128, 128, 4096])
    conv_jobs = []

    def half(name, src):
        shp = list(src.shape)
        dst = nc.dram_tensor(name + "_h", shp, BF16, kind="Internal").ap()
        letters = "abcdefg"[:len(shp)]
        pat = " ".join(letters) + " -> (" + " ".join(letters) + ")"
        sf = src.rearrange(pat).rearrange("(n p a b) -> n p a b", p=128, a=2, b=2048)
        df = dst.rearrange(pat).rearrange("(n p a b) -> n p a b", p=128, a=2, b=2048)
        for i in range(sf.shape[0]):
            conv_jobs.append((sf[i], df[i]))
        return dst

    w_in_h = half("w_in_b", w_in_d)
    w_pool_h = half("w_pool_b", w_pool_d)
    w_glu_h = half("w_glu_b", w_glu_d)
    w_out_h = half("w_out_b", w_out_d)
    w_q_h = half("w_q_b", w_q_d)
    ut_h = half("UTb", ut_d)
    v_h = half("Vn", v_d)
    w_pg_h = half("w_pg_b", w_pg_d)
    w_ple_h = half("w_ple_b", w_ple_d)
    cstate = {"i": 0}

    def conv_pump(n):
        while n > 0 and cstate["i"] < len(conv_jobs):
            sj, dj = conv_jobs[cstate["i"]]
            cstate["i"] += 1
            n -= 1
            so = k.csems[k.ci % len(k.csems)]
            k.ci += 1
            k.dma(pool, dj, sj, so=so)

    yT_d = dout("yT", [32, 128, NTOK])
    pool_o = dout("pool_o", [3, 128, 16, 16])
    ssm_o = dout("ssm_o", [3, 128, 2, 64])

    sb = k.sb
    vecs = sb(es, "vecs", [128, 176], F32)
    vB = Buf()
    ident = sb(es, "ident", [128, 128], F32)
    idB = Buf()
    ones = sb(es, "ones", [128, 128], F32)
    onB = Buf()
    rmask = sb(es, "rmask", [128, 4], F32)
    rmB = Buf()
    rcorr = sb(es, "rcorr", [128, 4, 16], F32)
    rcB = Buf()
    sq = [sb(es, "sq%d" % i, [128, NT], F32) for i in range(2)]
    sqB = bufs(2)
    rstd = sb(es, "rstd", [128, NT], F32)
    rsB = Buf()
    halo = sb(es, "halo", [128, 16, 16], F32)
    haB = Buf()
    hcar = sb(es, "hcar", [128, 2, 64], F32)
    hcB = [Buf(), Buf()]
    LRR = sb(es, "LRR", [128, 2, 64], F32)
    LI2 = sb(es, "LI2", [128, 2, 64], F32)
    LRRb = sb(es, "LRRb", [128, 2, 64], F32)
    LI2b = sb(es, "LI2b", [128, 2, 64], F32)
    lamB = Buf()
    Bq = sb(es, "Bq", [128, 16, 2, 128], BF16)
    BqB = Buf()
    Cz = sb(es, "Cz", [128, 64, 2, 128], BF16)
    CzB = Buf()
    Dz = sb(es, "Dz", [128, 16, 128], BF16)
    DzB = Buf()
    epsT = sb(es, "epsT", [128, 1], F32)
    epB = Buf()
    GM, GF, GP, GL, PSC, BGL, DSK = 0, 32, 64, 96, 128, 144, 160

    k.dma(sp, vecs[:], vecs_d[:, :], writes=(vB,))
    k.dma(sp, ident[:], ident_d[:, :], writes=(idB,))
    k.dma(sp, rmask[:], rmask_d[:, :], writes=(rmB,))
    k.dma(sp, rcorr[:], rcorr_d[:, :, :], writes=(rcB,))
    k.op(dve, lambda: nc.vector.memset(ones[:], 1.0), writes=(onB,))
    k.op(dve, lambda: nc.vector.memset(epsT[:], EPS), writes=(epB,))
    k.op(dve, lambda: nc.vector.memset(halo[:], 0.0), writes=(haB,))
    k.op(dve, lambda: nc.vector.memset(hcar[:], 0.0), writes=(hcB[0], hcB[1]))

    conv_pump(32)
    with ExitStack() as ph:
        lp = sb(ph, "lp", [128, 3, NLAM], F32)
        lpB = Buf()
        k.dma(sp, lp[:], lamp_d[:, :, :], writes=(lpB,))
        W = NLAM
        tl = [sb(ph, "tl%d" % i, [128, W], F32) for i in range(8)]
        ti = sb(ph, "ti", [128, W], I32)
        tB = bufs(9)
        dtT, erT, thT, lrT, liT, t5, t6, t7 = tl
        k.op(act, lambda: nc.scalar.activation(out=dtT[:], in_=lp[:, 2, :], func=AF.Exp), reads=(lpB,), writes=(tB[0],))
        k.op(dve, lambda: nc.vector.tensor_tensor(out=erT[:], in0=lp[:, 0, :], in1=dtT[:], op=ALU.mult), reads=(lpB, tB[0]), writes=(tB[1],))
        k.op(act, lambda: nc.scalar.activation(out=erT[:], in_=erT[:], func=AF.Exp), reads=(tB[1],), writes=(tB[1],))
        k.op(dve, lambda: nc.vector.tensor_tensor(out=thT[:], in0=lp[:, 1, :], in1=dtT[:], op=ALU.mult), reads=(lpB, tB[0]), writes=(tB[2],))

        def sin_rr(dst, dB, shift):
            k.op(dve, lambda: nc.vector.tensor_scalar(out=t5[:], in0=thT[:], scalar1=shift, scalar2=None, op0=ALU.add), reads=(tB[2],), writes=(tB[5],))
            k.op(dve, lambda: nc.vector.tensor_scalar(out=t6[:], in0=t5[:], scalar1=1.0 / TWO_PI, scalar2=0.5, op0=ALU.mult, op1=ALU.add), reads=(tB[5],), writes=(tB[6],))
            k.op(dve, lambda: nc.vector.tensor_copy(out=ti[:], in_=t6[:]), reads=(tB[6],), writes=(tB[8],))
            k.op(dve, lambda: nc.vector.tensor_copy(out=t6[:], in_=ti[:]), reads=(tB[8],), writes=(tB[6],))
            k.op(dve, lambda: nc.vector.scalar_tensor_tensor(out=t5[:], in0=t6[:], scalar=-TWO_PI, in1=t5[:], op0=ALU.mult, op1=ALU.add), reads=(tB[6], tB[5]), writes=(tB[5],))
            k.op(dve, lambda: nc.vector.tensor_scalar(out=t6[:], in0=t5[:], scalar1=-math.pi, scalar2=TWO_PI, op0=ALU.is_lt, op1=ALU.mult), reads=(tB[5],), writes=(tB[6],))
            k.op(dve, lambda: nc.vector.tensor_tensor(out=t5[:], in0=t5[:], in1=t6[:], op=ALU.add), reads=(tB[5], tB[6]), writes=(tB[5],))
            k.op(dve, lambda: nc.vector.tensor_scalar(out=t6[:], in0=t5[:], scalar1=math.pi, scalar2=-TWO_PI, op0=ALU.is_gt, op1=ALU.mult), reads=(tB[5],), writes=(tB[6],))
            k.op(dve, lambda: nc.vector.tensor_tensor(out=t5[:], in0=t5[:], in1=t6[:], op=ALU.add), reads=(tB[5], tB[6]), writes=(tB[5],))
            k.op(dve, lambda: nc.vector.tensor_scalar(out=t5[:], in0=t5[:], scalar1=-3.14159, scalar2=3.14159, op0=ALU.max, op1=ALU.min), reads=(tB[5],), writes=(tB[5],))
            k.op(act, lambda: nc.scalar.activation(out=dst[:], in_=t5[:], func=AF.Sin), reads=(tB[5],), writes=(dB,))

        sin_rr(liT, tB[4], 0.0)
        sin_rr(lrT, tB[3], math.pi / 2)
        k.op(dve, lambda: nc.vector.tensor_tensor(out=liT[:], in0=liT[:], in1=erT[:], op=ALU.mult), reads=(tB[4], tB[1]), writes=(tB[4],))
        k.op(dve, lambda: nc.vector.tensor_tensor(out=lrT[:], in0=lrT[:], in1=erT[:], op=ALU.mult), reads=(tB[3], tB[1]), writes=(tB[3],))
        k.op(dve, lambda: nc.vector.tensor_copy(out=LRR[:, 0, :], in_=lrT[:, 0:64]), reads=(tB[3],), writes=(lamB,))
        k.op(dve, lambda: nc.vector.tensor_copy(out=LRR[:, 1, :], in_=lrT[:, 0:64]), reads=(tB[3],), writes=(lamB,))
        k.op(dve, lambda: nc.vector.tensor_scalar(out=LI2[:, 0, :], in0=liT[:, 0:64], scalar1=-1.0, scalar2=None, op0=ALU.mult), reads=(tB[4],), writes=(lamB,))
        k.op(dve, lambda: nc.vector.tensor_copy(out=LI2[:, 1, :], in_=liT[:, 0:64]), reads=(tB[4],), writes=(lamB,))
        k.op(dve, lambda: nc.vector.tensor_scalar(out=t5[:], in0=lrT[:], scalar1=-1.0, scalar2=None, op0=ALU.add), reads=(tB[3],), writes=(tB[5],))
        k.op(dve, lambda: nc.vector.tensor_tensor(out=t6[:], in0=lp[:, 0, :], in1=lp[:, 0, :], op=ALU.mult), reads=(lpB,), writes=(tB[6],))
        k.op(dve, lambda: nc.vector.tensor_tensor(out=t7[:], in0=lp[:, 1, :], in1=lp[:, 1, :], op=ALU.mult), reads=(lpB,), writes=(tB[7],))
        k.op(dve, lambda: nc.vector.tensor_tensor(out=t6[:], in0=t6[:], in1=t7[:], op=ALU.add), reads=(tB[6], tB[7]), writes=(tB[6],))
        k.op(dve, lambda: nc.vector.reciprocal(out=t6[:], in_=t6[:]), reads=(tB[6],), writes=(tB[6],))
        k.op(dve, lambda: nc.vector.tensor_tensor(out=dtT[:], in0=t5[:], in1=lp[:, 0, :], op=ALU.mult), reads=(tB[5], lpB), writes=(tB[0],))
        k.op(dve, lambda: nc.vector.tensor_tensor(out=t7[:], in0=liT[:], in1=lp[:, 1, :], op=ALU.mult), reads=(tB[4], lpB), writes=(tB[7],))
        k.op(dve, lambda: nc.vector.tensor_tensor(out=dtT[:], in0=dtT[:], in1=t7[:], op=ALU.add), reads=(tB[0], tB[7]), writes=(tB[0],))
        k.op(dve, lambda: nc.vector.tensor_tensor(out=dtT[:], in0=dtT[:], in1=t6[:], op=ALU.mult), reads=(tB[0], tB[6]), writes=(tB[0],))
        k.op(dve, lambda: nc.vector.tensor_tensor(out=erT[:], in0=liT[:], in1=lp[:, 0, :], op=ALU.mult), reads=(tB[4], lpB), writes=(tB[1],))
        k.op(dve, lambda: nc.vector.tensor_tensor(out=t7[:], in0=t5[:], in1=lp[:, 1, :], op=ALU.mult), reads=(tB[5], lpB), writes=(tB[7],))
        k.op(dve, lambda: nc.vector.tensor_tensor(out=erT[:], in0=erT[:], in1=t7[:], op=ALU.subtract), reads=(tB[1], tB[7]), writes=(tB[1],))
        k.op(dve, lambda: nc.vector.tensor_tensor(out=erT[:], in0=erT[:], in1=t6[:], op=ALU.mult), reads=(tB[1], tB[6]), writes=(tB[1],))
        bqr = sb(ph, "bqr", [128, 2048], F32)
        bqi = sb(ph, "bqi", [128, 2048], F32)
        bqB = bufs(2)
        k.dma(sp, bqr[:], bq_d[0].rearrange("p a b -> p (a b)"), writes=(bqB[0],))
        k.dma(sp, bqi[:], bq_d[1].rearrange("p a b -> p (a b)"), writes=(bqB[1],))
        cr = dtT[:, 64:NLAM]
        ci = erT[:, 64:NLAM]
        a5 = t5[:, 0:2048]
        a7 = t7[:, 0:2048]
        Bre = thT[:, 0:2048]
        Bim = t6[:, 0:2048]
        r3 = lambda ap: ap.rearrange("p (a b) -> p a b", a=16, b=128)
        k.op(dve, lambda: nc.vector.tensor_tensor(out=a5, in0=cr, in1=bqr[:], op=ALU.mult), reads=(tB[0], bqB[0]), writes=(tB[5],))
        k.op(dve, lambda: nc.vector.tensor_tensor(out=a7, in0=ci, in1=bqi[:], op=ALU.mult), reads=(tB[1], bqB[1]), writes=(tB[7],))
        k.op(dve, lambda: nc.vector.tensor_tensor(out=Bre, in0=a5, in1=a7, op=ALU.subtract), reads=(tB[5], tB[7]), writes=(tB[2],))
        k.op(dve, lambda: nc.vector.tensor_tensor(out=a5, in0=cr, in1=bqi[:], op=ALU.mult), reads=(tB[0], bqB[1]), writes=(tB[5],))
        k.op(dve, lambda: nc.vector.tensor_tensor(out=a7, in0=ci, in1=bqr[:], op=ALU.mult), reads=(tB[1], bqB[0]), writes=(tB[7],))
        k.op(dve, lambda: nc.vector.tensor_tensor(out=Bim, in0=a5, in1=a7, op=ALU.add), reads=(tB[5], tB[7]), writes=(tB[6],))
        k.op(dve, lambda: nc.vector.tensor_copy(out=Bq[:, :, 0, :], in_=r3(Bre)), reads=(tB[2],), writes=(BqB,))
        k.op(dve, lambda: nc.vector.tensor_copy(out=Bq[:, :, 1, :], in_=r3(Bim)), reads=(tB[6],), writes=(BqB,))
        BqP = Cz[:].rearrange("p g r m -> p (g r m)").rearrange("p (a f r m) -> p a f r m", a=LB, f=16, r=2, m=128)
        Pre, Pim, tA_ = lp[:, 0, :], lp[:, 1, :], lp[:, 2, :]
        pwB = bufs(3)
        k.op(dve, lambda: nc.vector.memset(Pre, 1.0), reads=(tB[0], tB[1]), writes=(pwB[0], lpB))
        k.op(dve, lambda: nc.vector.memset(Pim, 0.0), writes=(pwB[1], lpB))
        for e_ in range(LB):
            a_ = LB - 1 - e_
            k.op(dve, lambda: nc.vector.tensor_tensor(out=a5, in0=Pre[:, 64:NLAM], in1=Bre, op=ALU.mult), reads=(pwB[0], tB[2]), writes=(tB[5],))
            k.op(dve, lambda: nc.vector.tensor_tensor(out=a7, in0=Pim[:, 64:NLAM], in1=Bim, op=ALU.mult), reads=(pwB[1], tB[6]), writes=(tB[7],))
            k.op(dve, lambda a_=a_: nc.vector.tensor_tensor(out=BqP[:, a_, :, 0, :], in0=r3(a5), in1=r3(a7), op=ALU.subtract), reads=(tB[5], tB[7]), writes=(CzB,))
            k.op(dve, lambda: nc.vector.tensor_tensor(out=a5, in0=Pre[:, 64:NLAM], in1=Bim, op=ALU.mult), reads=(pwB[0], tB[6], CzB), writes=(tB[5],))
            k.op(dve, lambda: nc.vector.tensor_tensor(out=a7, in0=Pim[:, 64:NLAM], in1=Bre, op=ALU.mult), reads=(pwB[1], tB[2], CzB), writes=(tB[7],))
            k.op(dve, lambda a_=a_: nc.vector.tensor_tensor(out=BqP[:, a_, :, 1, :], in0=r3(a5), in1=r3(a7), op=ALU.add), reads=(tB[5], tB[7]), writes=(CzB,))
            k.op(dve, lambda: nc.vector.tensor_tensor(out=tA_, in0=Pre, in1=lrT[:], op=ALU.mult), reads=(pwB[0], tB[3]), writes=(pwB[2],))
            k.op(dve, lambda: nc.vector.tensor_tensor(out=t5[:], in0=Pim, in1=liT[:], op=ALU.mult), reads=(pwB[1], tB[4], CzB), writes=(tB[5],))
            k.op(dve, lambda: nc.vector.tensor_tensor(out=tA_, in0=tA_, in1=t5[:], op=ALU.subtract), reads=(pwB[2], tB[5]), writes=(pwB[2],))
            k.op(dve, lambda: nc.vector.tensor_tensor(out=t5[:], in0=Pre, in1=liT[:], op=ALU.mult), reads=(pwB[0], tB[4], pwB[2]), writes=(tB[5],))
            k.op(dve, lambda: nc.vector.tensor_tensor(out=t7[:], in0=Pim, in1=lrT[:], op=ALU.mult), reads=(pwB[1], tB[3], CzB), writes=(tB[7],))
            k.op(dve, lambda: nc.vector.tensor_tensor(out=Pim, in0=t5[:], in1=t7[:], op=ALU.add), reads=(tB[5], tB[7]), writes=(pwB[1],))
            k.op(dve, lambda: nc.vector.tensor_copy(out=Pre, in_=tA_), reads=(pwB[2],), writes=(pwB[0],))
        k.op(dve, lambda: nc.vector.tensor_copy(out=LRRb[:, 0, :], in_=Pre[:, 0:64]), reads=(pwB[0],), writes=(lamB,))
        k.op(dve, lambda: nc.vector.tensor_copy(out=LRRb[:, 1, :], in_=Pre[:, 0:64]), reads=(pwB[0],), writes=(lamB,))
        k.op(dve, lambda: nc.vector.tensor_scalar(out=LI2b[:, 0, :], in0=Pim[:, 0:64], scalar1=-1.0, scalar2=None, op0=ALU.mult), reads=(pwB[1],), writes=(lamB,))
        k.op(dve, lambda: nc.vector.tensor_copy(out=LI2b[:, 1, :], in_=Pim[:, 0:64]), reads=(pwB[1],), writes=(lamB,))
        for fc in range(16):
            k.op(dve, lambda fc=fc: nc.vector.tensor_scalar(out=Dz[:, fc, :], in0=ident[:], scalar1=vecs[:, DSK + fc:DSK + fc + 1], scalar2=None, op0=ALU.mult), reads=(idB, vB), writes=(DzB,))
        k.barrier()

    k.init_slots()
    xT = sb(es, "xT", [128, 32, NT], F32)
    xB = bufs(32)

    def load_x(src_d, t0, N):
        for q in range(4):
            k.dma(sp, xT[:, q * 8:(q + 1) * 8, 0:N], src_d[q * 8:(q + 1) * 8, :, t0:t0 + N].rearrange("c p t -> p c t"),
                  writes=tuple(xB[q * 8:(q + 1) * 8]))

    def rmsnorm(N, goff, emit):
        pt, pb = k.psum()
        for c in range(32):
            s, sB_ = sq[c % 2], sqB[c % 2]
            k.op(act, lambda c=c, s=s: nc.scalar.activation(out=s[:, 0:N], in_=xT[:, c, 0:N], func=AF.Square), reads=(xB[c],), writes=(sB_,))
            k.op(pe, lambda c=c, s=s: nc.tensor.matmul(pt[:, 0:N], lhsT=ones[:], rhs=s[:, 0:N], start=(c == 0), stop=(c == 31)), reads=(onB, sB_), writes=(pb,))
        k.op(act, lambda: nc.scalar.activation(out=rstd[:, 0:N], in_=pt[:, 0:N], func=AF.Sqrt, bias=epsT[:], scale=1.0 / D), reads=(pb, epB), writes=(rsB,))
        k.op(dve, lambda: nc.vector.reciprocal(out=rstd[:, 0:N], in_=rstd[:, 0:N]), reads=(rsB,), writes=(rsB,))
        for c in range(32):
            o_ap, o_buf, post = emit(c)
            k.op(dve, lambda c=c, o_ap=o_ap: nc.vector.scalar_tensor_tensor(out=o_ap, in0=xT[:, c, 0:N], scalar=vecs[:, goff + c:goff + c + 1], in1=rstd[:, 0:N], op0=ALU.mult, op1=ALU.mult),
                 reads=(xB[c], vB, rsB), writes=(o_buf,))
            if post:
                post(c)

    def load_cz():
        k.barrier()
        for h in range(4):
            k.dma(pool, Cz[:, h * 16:(h + 1) * 16, 0, :], cz_d[0][:, h * 16:(h + 1) * 16, :], writes=(CzB,))
            k.dma(pool, Cz[:, h * 16:(h + 1) * 16, 1, :], cz_d[1][:, h * 16:(h + 1) * 16, :], writes=(CzB,))
        k.op(dve, lambda: nc.vector.tensor_scalar(out=Cz[:, :, 1, :], in0=Cz[:, :, 1, :], scalar1=-1.0, scalar2=None, op0=ALU.mult), reads=(CzB,), writes=(CzB,))
        k.barrier()

    def mixer_pre_all():
        N = NT
        HN = 128
        NBK = HN // LB
        BqP = Cz[:].rearrange("p g r m -> p (g r m)").rearrange("p (a f r m) -> p a f r m", a=LB, f=16, r=2, m=128)
        with ExitStack() as ph:
            actA = sb(ph, "actA", [128, 32, NT], BF16)
            aB = bufs(32)
            uT2 = [sb(ph, "uTp%d" % i, [128, 16, NT], BF16) for i in range(2)]
            uB2 = [bufs(16), bufs(16)]
            uTm2 = [sb(ph, "uTmP%d" % i, [128, 4, 16, HN], BF16) for i in range(1)] * 2
            umB2 = bufs(1) * 2
            BUc2 = [sb(ph, "BUc%d" % i, [128, NBK, 2, 64], F32) for i in range(2)]
            BUB2 = bufs(2)
            HS = sb(ph, "HSp", [128, 2, 2, 64], F32)
            HSB = [Buf(), Buf()]
            P1 = sb(ph, "P1p", [128, 2, 64], F32)
            P2 = sb(ph, "P2p", [128, 2, 64], F32)
            PB = [(Buf(), Buf()), (Buf(), Buf())]
            halves = [(dve, nc.vector, 0, 32), (pool, nc.gpsimd, 32, 64)]
            def stage_a1(ist):
                conv_pump(16)
                load_x(xpre_d, ist * NT, N)
                rmsnorm(N, GM, lambda c: (actA[:, c, 0:N], aB[c], None))

            def stage_a2(ist):
                uT, uB = uT2[ist % 2], uB2[ist % 2]

                def evac_in(mc, pt, pb):
                    k.op(act, lambda: nc.scalar.copy(out=uT[:, mc - 16, 0:N], in_=pt[:, 0:N]), reads=(pb,), writes=(uB[mc - 16],))
                k.linear(w_in_h, 32, 4, [4, 5, 6, 7], lambda kc: (actA[:, kc, 0:N], aB[kc]), N, evac_in)

            def stage_b1(ist):
                uT, uB = uT2[ist % 2], uB2[ist % 2]
                for hf in range(N // HN):
                    c0 = hf * HN
                    uTm, umB, BUc, BUB = uTm2[hf % 2], umB2[hf % 2], BUc2[hf % 2], BUB2[hf % 2]
                    for q in range(4):
                        k.op(act, lambda q=q: nc.scalar.activation(out=uTm[:, q, :, :], in_=uT[:, :, c0:c0 + HN], func=AF.Copy, scale=rmask[:, q:q + 1]), reads=tuple(uB) + (rmB,), writes=(umB,))
                    for bk in range(8):
                        pt, pb = k.psum()
                        for gl_ in range(8):
                            gj = bk * 8 + gl_
                            fc, q = gj // 4, gj % 4
                            for ri in range(2):
                                col = (gl_ * 2 + ri) * NBK
                                for a_ in range(LB):
                                    k.op(pe, lambda pt=pt, fc=fc, q=q, ri=ri, col=col, a_=a_: nc.tensor.matmul(
                                        pt[:, col:col + NBK], lhsT=BqP[:, a_, fc, ri, :], rhs=uTm[:, q, fc, :].rearrange("p (j a) -> p a j", a=LB)[:, a_, :],
                                        start=(a_ == 0), stop=(a_ == LB - 1)), reads=(CzB, umB), writes=(pb,))
                        k.op(act, lambda pt=pt, bk=bk: nc.scalar.copy(out=BUc[:, :, :, bk * 8:(bk + 1) * 8], in_=pt[:, 0:512].rearrange("p (g r j) -> p j r g", g=8, r=2, j=NBK)), reads=(pb,), writes=(BUB,))

            def stage_b2(ist):
                for hi, (eng, eo, g0, g1) in enumerate(halves):
                    k.op(eng, lambda eo=eo, g0=g0, g1=g1: eo.tensor_copy(out=HS[:, 0, :, g0:g1], in_=hcar[:, :, g0:g1]), reads=(hcB[hi],), writes=(HSB[hi],))
                cur = 0
                for hf in range(N // HN):
                    BUc, BUB = BUc2[hf % 2], BUB2[hf % 2]
                    for j in range(NBK):
                        nxt = 1 - cur
                        for hi, (eng, eo, g0, g1) in enumerate(halves):
                            p1B, p2B = PB[hi]
                            k.op(eng, lambda eo=eo, g0=g0, g1=g1: eo.tensor_tensor(out=P1[:, :, g0:g1], in0=HS[:, cur, :, g0:g1], in1=LRRb[:, :, g0:g1], op=ALU.mult), reads=(HSB[hi], lamB), writes=(p1B,))
                            k.op(eng, lambda eo=eo, g0=g0, g1=g1: eo.tensor_tensor(out=P2[:, :, g0:g1], in0=HS[:, cur, ::-1, g0:g1], in1=LI2b[:, :, g0:g1], op=ALU.mult), reads=(HSB[hi], lamB), writes=(p2B,))
                            k.op(eng, lambda eo=eo, g0=g0, g1=g1: eo.tensor_tensor(out=P1[:, :, g0:g1], in0=P1[:, :, g0:g1], in1=P2[:, :, g0:g1], op=ALU.add), reads=(p1B, p2B), writes=(p1B,))
                            k.op(eng, lambda eo=eo, g0=g0, g1=g1, j=j: eo.tensor_tensor(out=HS[:, nxt, :, g0:g1], in0=P1[:, :, g0:g1], in1=BUc[:, j, :, g0:g1], op=ALU.add), reads=(p1B, BUB), writes=(HSB[hi],))
                        cur = nxt
                for hi, (eng, eo, g0, g1) in enumerate(halves):
                    k.op(eng, lambda eo=eo, g0=g0, g1=g1: eo.tensor_copy(out=hcar[:, :, g0:g1], in_=HS[:, cur, :, g0:g1]), reads=(HSB[hi],), writes=(hcB[hi],))

            nst = PRE // NT
            stage_a1(0)
            stage_a2(0)
            for ist in range(nst):
                if ist + 1 < nst:
                    stage_a1(ist + 1)
                stage_b1(ist)
                if ist + 1 < nst:
                    stage_a2(ist + 1)
                stage_b2(ist)
            conv_pump(10 ** 6)
            k.barrier()

    def mixer(mode, src_d, t0, N, out_idx=None):
        with ExitStack() as ph:
            actA = sb(ph, "actA", [128, 32, NT], BF16)
            aB = bufs(32)
            uT = sb(ph, "uT", [128, 16, NT], BF16)
            uB = bufs(16)
            sT = sb(ph, "sT", [128, 16, NT], BF16)
            sB = bufs(16)
            sg = [sb(ph, "sg%d" % i, [128, NT], F32) for i in range(2)]
            sgB = bufs(2)
            php = ExitStack()
            load_x(src_d, t0, N)
            rmsnorm(N, GM, lambda c: (actA[:, c, 0:N], aB[c], None))
            do_pool = mode in ("halo", "main", "sample")
            do_ssm = mode in ("pre", "main", "sample")
            if do_pool:
                zp = sb(php, "zp", [128, 16, 16 + NT], F32)
                zB = bufs(16)
                tA = sb(php, "tA", [128, 4, 16 + NT], F32)
                tBt = sb(php, "tB", [128, 4, 16 + NT], F32)
                tAB, tBB = Buf(), Buf()
            if mode == "sample":
                segs = [(0, 64, 16), (64, 64, 96)]
                Wd = 160
            else:
                segs = [(0, N, 16)]
                Wd = 16 + N
            if mode in ("main",):
                k.op(act, lambda: nc.scalar.copy(out=zp[:, :, 0:16], in_=halo[:]), reads=(haB,), writes=tuple(zB))
            if mode == "sample":
                for s in range(2):
                    k.dma(sp, zp[:, :, s * 80:s * 80 + 16], cp_d[s], writes=tuple(zB))

            def evac_in(mc, pt, pb):
                if mc < 16:
                    for (ts, tl_, cs) in segs:
                        k.op(act, lambda ts=ts, tl_=tl_, cs=cs: nc.scalar.copy(out=zp[:, mc, cs:cs + tl_], in_=pt[:, ts:ts + tl_]), reads=(pb,), writes=(zB[mc],))
                else:
                    k.op(act, lambda: nc.scalar.copy(out=uT[:, mc - 16, 0:N], in_=pt[:, 0:N]), reads=(pb,), writes=(uB[mc - 16],))

            mgs = []
            if do_pool:
                mgs += [0, 1, 2, 3]
            if do_ssm:
                mgs += [4, 5, 6, 7]
            k.linear(w_in_h, 32, 4, mgs, lambda kc: (actA[:, kc, 0:N], aB[kc]), N, evac_in)

            if mode == "halo":
                k.op(act, lambda: nc.scalar.copy(out=halo[:], in_=zp[:, :, N:N + 16]), reads=tuple(zB), writes=(haB,))
                k.barrier()
                php.close()
                return

            if do_pool:
                for g in range(4):
                    w = 2 << g
                    zg = zp[:, g * 4:(g + 1) * 4, :]
                    zgB = tuple(zB[g * 4:(g + 1) * 4])
                    k.op(dve, lambda zg=zg: nc.vector.tensor_tensor(out=tA[:, :, 1:Wd], in0=zg[:, :, 1:Wd], in1=zg[:, :, 0:Wd - 1], op=ALU.add), reads=zgB, writes=(tAB,))
                    cur, curB, oth, othB = tA, tAB, tBt, tBB
                    off = 1
                    stp = 2
                    while stp < w:
                        no = off + stp
                        k.op(dve, lambda cur=cur, oth=oth, no=no, off=off, stp=stp: nc.vector.tensor_tensor(out=oth[:, :, no:Wd], in0=cur[:, :, no:Wd], in1=cur[:, :, off:Wd - stp], op=ALU.add), reads=(curB,), writes=(othB,))
                        cur, curB, oth, othB = oth, othB, cur, curB
                        off = no
                        stp *= 2
                    if mode == "main" and t0 == 0:
                        k.op(dve, lambda cur=cur, g=g: nc.vector.tensor_tensor(out=cur[:, :, 16:32], in0=cur[:, :, 16:32], in1=rcorr[:, g, :].unsqueeze(1).broadcast_to([128, 4, 16]), op=ALU.mult), reads=(curB, rcB), writes=(curB,))
                    for (ts, tl_, cs) in segs:
                        k.op(dve, lambda cur=cur, zg=zg, ts=ts, tl_=tl_, cs=cs, g=g, w=w: nc.vector.scalar_tensor_tensor(
                            out=actA[:, 16 + g * 4:16 + (g + 1) * 4, ts:ts + tl_], in0=cur[:, :, cs:cs + tl_], scalar=1.0 / w, in1=zg[:, :, cs:cs + tl_], op0=ALU.mult, op1=ALU.subtract),
                            reads=(curB,) + zgB, writes=tuple(aB[16 + g * 4:16 + (g + 1) * 4]))
                if mode == "main":
                    k.op(act, lambda: nc.scalar.copy(out=halo[:], in_=zp[:, :, N:N + 16]), reads=tuple(zB), writes=(haB,))
                    if out_idx is not None:
                        k.dma(sp, pool_o[0], zp[:, :, N:N + 16], reads=tuple(zB))
                if mode == "sample":
                    for s in range(2):
                        k.dma(sp, pool_o[1 + s], zp[:, :, s * 80 + 64:s * 80 + 80], reads=tuple(zB))
                for g in range(4):
                    def evac_p(mc, pt, pb, g=g):
                        c = g * 4 + mc
                        k.op(act, lambda: nc.scalar.activation(out=actA[:, c, 0:N], in_=pt[:, 0:N], func=AF.Copy, scale=vecs[:, PSC + c:PSC + c + 1]), reads=(pb, vB), writes=(aB[c],))
                    k.linear(w_pool_h[g:g + 1], 4, 4, [0], lambda kc, g=g: (actA[:, 16 + g * 4 + kc, 0:N], aB[16 + g * 4 + kc]), N, evac_p)

            k.barrier()
            php.close()
            phs = ExitStack()
            BU2 = [sb(phs, "BU%d" % i, [128, LC, 2, 64], F32) for i in range(2)]
            BUB2 = bufs(2)
            HS2 = [sb(phs, "HS%d" % i, [128, LC + 1, 2, 64], F32) for i in range(2)]
            HSB2 = [[Buf(), Buf()], [Buf(), Buf()]]
            HSb2 = [sb(phs, "HSb%d" % i, [128, LC, 2, 64], BF16) for i in range(2)]
            HbB2 = bufs(2)
            uTm2 = [sb(phs, "uTm%d" % i, [128, 4, 16, LC], BF16) for i in range(2)]
            umB2 = bufs(2)
            P1 = sb(phs, "P1", [128, 2, 64], F32)
            P2 = sb(phs, "P2", [128, 2, 64], F32)
            PB = [(Buf(), Buf()), (Buf(), Buf())]
            halves = [(dve, nc.vector, 0, 32), (pool, nc.gpsimd, 32, 64)]
            nch = N // LC
            for ch in range(nch):
                c0 = ch * LC
                b_ = ch % 2
                BU, BUB, HS, HSB, HSb, HbB, uTm, umB = BU2[b_], BUB2[b_], HS2[b_], HSB2[b_], HSb2[b_], HbB2[b_], uTm2[b_], umB2[b_]
                HSp, HSpB = HS2[1 - b_], HSB2[1 - b_]
                if mode == "sample" and c0 % 64 == 0:
                    s_ = c0 // 64
                    k.dma(sp, HS[:, 0, :, :], st0_d[s_], writes=(HSB[0], HSB[1]))
                elif ch == 0:
                    for hi, (eng, eo, g0, g1) in enumerate(halves):
                        k.op(eng, lambda eo=eo, g0=g0, g1=g1: eo.tensor_copy(out=HS[:, 0, :, g0:g1], in_=hcar[:, :, g0:g1]), reads=(hcB[hi],), writes=(HSB[hi],))
                else:
                    for hi, (eng, eo, g0, g1) in enumerate(halves):
                        k.op(eng, lambda eo=eo, g0=g0, g1=g1: eo.tensor_copy(out=HS[:, 0, :, g0:g1], in_=HSp[:, LC, :, g0:g1]), reads=(HSpB[hi],), writes=(HSB[hi],))
                for q in range(4):
                    k.op(act, lambda q=q: nc.scalar.activation(out=uTm[:, q, :, :], in_=uT[:, :, c0:c0 + LC], func=AF.Copy, scale=rmask[:, q:q + 1]), reads=tuple(uB) + (rmB,), writes=(umB,))
                for bk in range(4):
                    pt, pb = k.psum()
                    for gl_ in range(16):
                        gj = bk * 16 + gl_
                        fc, q = gj // 4, gj % 4
                        for ri in range(2):
                            col = (gl_ * 2 + ri) * LC
                            k.op(pe, lambda pt=pt, fc=fc, q=q, ri=ri, col=col: nc.tensor.matmul(pt[:, col:col + LC], lhsT=Bq[:, fc, ri, :], rhs=uTm[:, q, fc, :], start=True, stop=True),
                                 reads=(BqB, umB), writes=(pb,))
                    k.op(act, lambda pt=pt, bk=bk: nc.scalar.copy(out=BU[:, :, :, bk * 16:(bk + 1) * 16], in_=pt[:, 0:512].rearrange("p (g r t) -> p t r g", g=16, r=2, t=LC)), reads=(pb,), writes=(BUB,))
                for t in range(LC):
                    for hi, (eng, eo, g0, g1) in enumerate(halves):
                        p1B, p2B = PB[hi]
                        k.op(eng, lambda eo=eo, g0=g0, g1=g1, t=t: eo.tensor_tensor(out=P1[:, :, g0:g1], in0=HS[:, t, :, g0:g1], in1=LRR[:, :, g0:g1], op=ALU.mult), reads=(HSB[hi], lamB), writes=(p1B,))
                        if REV_AP:
                            k.op(eng, lambda eo=eo, g0=g0, g1=g1, t=t: eo.tensor_tensor(out=P2[:, :, g0:g1], in0=HS[:, t, ::-1, g0:g1], in1=LI2[:, :, g0:g1], op=ALU.mult), reads=(HSB[hi], lamB), writes=(p2B,))
                        else:
                            k.op(eng, lambda eo=eo, g0=g0, g1=g1, t=t: eo.tensor_tensor(out=P2[:, 0, g0:g1], in0=HS[:, t, 1, g0:g1], in1=LI2[:, 0, g0:g1], op=ALU.mult), reads=(HSB[hi], lamB), writes=(p2B,))
                            k.op(eng, lambda eo=eo, g0=g0, g1=g1, t=t: eo.tensor_tensor(out=P2[:, 1, g0:g1], in0=HS[:, t, 0, g0:g1], in1=LI2[:, 1, g0:g1], op=ALU.mult), reads=(HSB[hi], lamB), writes=(p2B,))
                        k.op(eng, lambda eo=eo, g0=g0, g1=g1, t=t: eo.tensor_tensor(out=P1[:, :, g0:g1], in0=P1[:, :, g0:g1], in1=P2[:, :, g0:g1], op=ALU.add), reads=(p1B, p2B), writes=(p1B,))
                        k.op(eng, lambda eo=eo, g0=g0, g1=g1, t=t: eo.tensor_tensor(out=HS[:, t + 1, :, g0:g1], in0=P1[:, :, g0:g1], in1=BU[:, t, :, g0:g1], op=ALU.add), reads=(p1B, BUB), writes=(HSB[hi],))
                k.op(act, lambda: nc.scalar.copy(out=HSb[:], in_=HS[:, 1:LC + 1, :, :]), reads=(HSB[0], HSB[1]), writes=(HbB,))
                pt, pb = k.psum()
                for fc in range(16):
                    ia = 0
                    for q in range(4):
                        gj = fc * 4 + q
                        for ri in range(2):
                            k.op(pe, lambda pt=pt, fc=fc, gj=gj, ri=ri, ia=ia: nc.tensor.matmul(pt[:, fc * LC:(fc + 1) * LC], lhsT=Cz[:, gj, ri, :], rhs=HSb[:, :, ri, gj], start=(ia == 0), stop=False),
                                 reads=(CzB, HbB), writes=(pb,))
                            ia += 1
                    k.op(pe, lambda pt=pt, fc=fc: nc.tensor.matmul(pt[:, fc * LC:(fc + 1) * LC], lhsT=Dz[:, fc, :], rhs=uT[:, fc, c0:c0 + LC], start=False, stop=True), reads=(DzB, uB[fc]), writes=(pb,))
                k.op(act, lambda pt=pt: nc.scalar.activation(out=sT[:, :, c0:c0 + LC], in_=pt[:, 0:16 * LC].rearrange("p (f t) -> p f t", f=16, t=LC), func=AF.Gelu), reads=(pb,), writes=tuple(sB))
                if mode == "sample" and (c0 + LC) % 64 == 0:
                    s_ = c0 // 64
                    k.dma(sp, ssm_o[1 + s_], HS[:, LC, :, :], reads=(HSB[0], HSB[1]))
                if ch == nch - 1 and mode == "main":
                    for hi, (eng, eo, g0, g1) in enumerate(halves):
                        k.op(eng, lambda eo=eo, g0=g0, g1=g1: eo.tensor_copy(out=hcar[:, :, g0:g1], in_=HS[:, LC, :, g0:g1]), reads=(HSB[hi],), writes=(hcB[hi],))
                    if out_idx is not None:
                        k.dma(sp, ssm_o[0], HS[:, LC, :, :], reads=(HSB[0], HSB[1]))

            def evac_glu(mc, pt, pb):
                s_, sB_ = sg[mc % 2], sgB[mc % 2]
                k.op(act, lambda: nc.scalar.activation(out=s_[:, 0:N], in_=pt[:, 0:N], func=AF.Sigmoid, bias=vecs[:, BGL + mc:BGL + mc + 1]), reads=(pb, vB), writes=(sB_,))
                k.op(dve, lambda: nc.vector.tensor_tensor(out=actA[:, 16 + mc, 0:N], in0=sT[:, mc, 0:N], in1=s_[:, 0:N], op=ALU.mult), reads=(sB[mc], sB_), writes=(aB[16 + mc],))
            k.linear(w_glu_h, 16, 4, [0, 1, 2, 3], lambda kc: (sT[:, kc, 0:N], sB[kc]), N, evac_glu)

            def evac_out(mc, pt, pb):
                k.op(dve, lambda: nc.vector.tensor_tensor(out=xT[:, mc, 0:N], in0=pt[:, 0:N], in1=xT[:, mc, 0:N], op=ALU.add), reads=(pb, xB[mc]), writes=(xB[mc],))
            k.linear(w_out_h, 32, 4, list(range(8)), lambda kc: (actA[:, kc, 0:N], aB[kc]), N, evac_out)
            k.barrier()
            phs.close()

    def peer(N):
        ntt = N // 128
        with ExitStack() as ph:
            xn = sb(ph, "xn", [128, 32, NT], BF16)
            xnB = bufs(32)
            eT = [sb(ph, "eT%d" % i, [128, 16, 128], F32) for i in range(ntt)]
            eB = bufs(ntt)
            Dh = [sb(ph, "Dh%d" % i, [128, 8, 128], BF16) for i in range(ntt)]
            DhB = bufs(ntt)
            th = [sb(ph, "th%d" % i, [128, 8], F32) for i in range(ntt)]
            thB = bufs(ntt)
            rmsnorm(N, GF, lambda c: (xn[:, c, 0:N], xnB[c], None))
            with ExitStack() as ph2:
                qT = sb(ph2, "qT", [128, 16, NT], BF16)
                qB = bufs(16)
                keys = sb(ph2, "keys", [128, 16, 128], BF16)
                kyB = Buf()
                k.dma(pool, keys[:], keys_d[:, :, :], writes=(kyB,))

                def evac_q(mc, pt, pb):
                    k.op(act, lambda: nc.scalar.copy(out=qT[:, mc, 0:N], in_=pt[:, 0:N]), reads=(pb,), writes=(qB[mc],))
                k.linear(w_q_h, 32, 4, [0, 1, 2, 3], lambda kc: (xn[:, kc, 0:N], xnB[kc]), N, evac_q)
                mx = sb(ph2, "mx", [128, 16], F32)
                mxB = Buf()
                T16 = sb(ph2, "T16", [128, 16, 16], F32)
                T16B = Buf()
                tmpe = sb(ph2, "tmpe", [128, 128], F32)
                tmpeB = Buf()
                cand = sb(ph2, "cand", [128, 8, 256], F32)
                cdB = Buf()
                tmpc = sb(ph2, "tmpc", [128, 256], F32)
                tcB = Buf()
                C16 = sb(ph2, "C16", [128, 8, 16], F32)
                C16B = Buf()
                Zt = sb(ph2, "Zt", [128, 8], F32)
                ZB = Buf()
                for tt in range(ntt):
                    pss = [k.psum() for _ in range(4)]
                    for hp in range(16):
                        pt, pb = pss[hp // 4]
                        k.op(pe, lambda pt=pt, hp=hp: nc.tensor.matmul(pt[:, (hp % 4) * 128:(hp % 4 + 1) * 128], lhsT=qT[:, hp, tt * 128:(tt + 1) * 128], rhs=keys[:, hp, :], start=True, stop=True),
                             reads=(qB[hp], kyB), writes=(pb,))
                    for b in range(4):
                        pt, pb = pss[b]
                        k.op(dve, lambda pt=pt, b=b: nc.vector.tensor_reduce(out=mx[:, b * 4:(b + 1) * 4], in_=pt[:, 0:512].rearrange("p (a n) -> p a n", a=4, n=128), axis=AX.X, op=ALU.max), reads=(pb,), writes=(mxB,))
                    k.op(dve, lambda: nc.vector.tensor_scalar(out=mx[:], in0=mx[:], scalar1=-1.0, scalar2=None, op0=ALU.mult), reads=(mxB,), writes=(mxB,))
                    for hp in range(16):
                        pt, pb = pss[hp // 4]
                        k.op(act, lambda pt=pt, hp=hp: nc.scalar.activation(out=eT[tt][:, hp, :], in_=pt[:, (hp % 4) * 128:(hp % 4 + 1) * 128], func=AF.Exp, bias=mx[:, hp:hp + 1]), reads=(pb, mxB), writes=(eB[tt],))
                    for hp in range(16):
                        k.op(dve, lambda hp=hp: nc.vector.max(out=T16[:, hp, 0:8], in_=eT[tt][:, hp, :]), reads=(eB[tt],), writes=(T16B,))
                        k.op(dve, lambda hp=hp: nc.vector.match_replace(out=tmpe[:], in_to_replace=T16[:, hp, 0:8], in_values=eT[tt][:, hp, :], imm_value=-1.0), reads=(eB[tt], T16B), writes=(tmpeB,))
                        k.op(dve, lambda hp=hp: nc.vector.max(out=T16[:, hp, 8:16], in_=tmpe[:]), reads=(tmpeB,), writes=(T16B,))
                    for h in range(8):
                        k.op(dve, lambda h=h: nc.vector.tensor_tensor(out=cand[:, h, :].rearrange("p (a b) -> p a b", a=16, b=16), in0=T16[:, 2 * h, :].unsqueeze(2).broadcast_to([128, 16, 16]),
                                                                      in1=T16[:, 2 * h + 1, :].unsqueeze(1).broadcast_to([128, 16, 16]), op=ALU.mult), reads=(T16B,), writes=(cdB,))
                        k.op(dve, lambda h=h: nc.vector.max(out=C16[:, h, 0:8], in_=cand[:, h, :]), reads=(cdB,), writes=(C16B,))
                        k.op(dve, lambda h=h: nc.vector.match_replace(out=tmpc[:], in_to_replace=C16[:, h, 0:8], in_values=cand[:, h, :], imm_value=-1.0), reads=(cdB, C16B), writes=(tcB,))
                        k.op(dve, lambda h=h: nc.vector.max(out=C16[:, h, 8:16], in_=tmpc[:]), reads=(tcB,), writes=(C16B,))
                    k.op(dve, lambda: nc.vector.tensor_reduce(out=Zt[:], in_=C16[:], axis=AX.X, op=ALU.add), reads=(C16B,), writes=(ZB,))
                    k.op(dve, lambda: nc.vector.reciprocal(out=Zt[:], in_=Zt[:]), reads=(ZB,), writes=(ZB,))
                    k.op(dve, lambda: nc.vector.tensor_copy(out=th[tt][:], in_=C16[:, :, 15]), reads=(C16B,), writes=(thB[tt],))
                    for h in range(8):
                        k.op(dve, lambda h=h: nc.vector.tensor_scalar(out=Dh[tt][:, h, :], in0=ident[:], scalar1=Zt[:, h:h + 1], scalar2=None, op0=ALU.mult), reads=(idB, ZB), writes=(DhB[tt],))
                k.barrier()

            NG = 32
            Gh = [[sb(ph, "Gh%d_%d" % (p_, i), [128, 8, 4, 128], BF16) for i in range(ntt)] for p_ in range(2)]
            GhB = [bufs(ntt) for _ in range(2)]
            Eb = [sb(ph, "Eb%d" % i, [128, 4, 128], F32) for i in range(2)]
            EbB = bufs(2)
            gl = sb(ph, "gl", [128, 2, 4, NT], BF16)
            glB = [bufs(4), bufs(4)]
            AT = sb(ph, "AT", [128, 2, 4, NT], BF16)
            ATB = [bufs(4), bufs(4)]
            cnt = {"ei": 0}

            def emit_G(eg, pieces=None):
                pp = eg % 2
                for tt in range(ntt):
                    for h in range(8):
                        if pieces is not None and (tt * 8 + h) not in pieces:
                            continue
                        E_, EB_ = Eb[cnt["ei"] % 2], EbB[cnt["ei"] % 2]
                        cnt["ei"] += 1
                        k.op(pool, lambda E_=E_, tt=tt, h=h: nc.gpsimd.tensor_tensor(out=E_[:], in0=eT[tt][:, 2 * h, eg * 4:(eg + 1) * 4].unsqueeze(2).broadcast_to([128, 4, 128]),
                                                                                   in1=eT[tt][:, 2 * h + 1, :].unsqueeze(1).broadcast_to([128, 4, 128]), op=ALU.mult), reads=(eB[tt],), writes=(EB_,))
                        k.op(dve, lambda E_=E_, tt=tt, h=h: nc.vector.scalar_tensor_tensor(out=Gh[pp][tt][:, h, :, :].rearrange("p a b -> p (a b)"), in0=E_[:].rearrange("p a b -> p (a b)"), scalar=th[tt][:, h:h + 1],
                                                                                         in1=E_[:].rearrange("p a b -> p (a b)"), op0=ALU.is_ge, op1=ALU.mult), reads=(EB_, thB[tt]), writes=(GhB[pp][tt],))

            items = []
            stt = {}
            for eg in range(NG):
                pp = eg % 2
                for j in range(4):
                    ec = eg * 4 + j
                    for half in range(2):
                        def cons_u(wt, wb, eg=eg, pp=pp, j=j, half=half):
                            if eg == 0 and j == 0 and half == 0:
                                emit_G(0)
                            if half == 0:
                                stt["ph"] = k.psum()
                            pt, pb = stt["ph"]
                            for dcl in range(16):
                                dc = half * 16 + dcl
                                k.op(pe, lambda pt=pt, dcl=dcl, dc=dc: nc.tensor.matmul(pt[:, 0:N], lhsT=wt[:, dcl * 128:(dcl + 1) * 128], rhs=xn[:, dc, 0:N], start=(dc == 0), stop=(dc == 31)),
                                     reads=(wb, xnB[dc]), writes=(pb,))
                            if half == 1:
                                k.op(act, lambda pt=pt: nc.scalar.activation(out=gl[:, pp, j, 0:N], in_=pt[:, 0:N], func=AF.Gelu), reads=(pb,), writes=(glB[pp][j],))
                                pg, pgb = k.psum()
                                for tt in range(ntt):
                                    for h in range(8):
                                        k.op(pe, lambda pg=pg, tt=tt, h=h: nc.tensor.matmul(pg[:, tt * 128:(tt + 1) * 128], lhsT=Gh[pp][tt][:, h, j, :], rhs=Dh[tt][:, h, :], start=(h == 0), stop=(h == 7)),
                                             reads=(GhB[pp][tt], DhB[tt]), writes=(pgb,))
                                k.op(dve, lambda pg=pg: nc.vector.tensor_tensor(out=AT[:, pp, j, 0:N], in0=pg[:, 0:N], in1=gl[:, pp, j, 0:N], op=ALU.mult), reads=(pgb, glB[pp][j]), writes=(ATB[pp][j],))
                        items.append((ut_h[ec, half], (16, 128), cons_u))
                for half in range(2):
                    for j in range(4):
                        ec = eg * 4 + j

                        def cons_v(wt, wb, eg=eg, pp=pp, j=j, half=half):
                            if j == 0:
                                stt["v"] = []
                            stt["v"].append((wt, wb))
                            if j == 3:
                                vs = stt["v"]
                                for dcl in range(16):
                                    dc = half * 16 + dcl
                                    pt, pb = k.psum()
                                    for jj in range(4):
                                        vt, vb = vs[jj]
                                        k.op(pe, lambda pt=pt, vt=vt, jj=jj, dcl=dcl: nc.tensor.matmul(pt[:, 0:N], lhsT=vt[:, dcl * 128:(dcl + 1) * 128], rhs=AT[:, pp, jj, 0:N], start=(jj == 0), stop=(jj == 3)),
                                             reads=(vb, ATB[pp][jj]), writes=(pb,))
                                    k.op(dve, lambda pt=pt, dc=dc: nc.vector.tensor_tensor(out=xT[:, dc, 0:N], in0=pt[:, 0:N], in1=xT[:, dc, 0:N], op=ALU.add), reads=(pb, xB[dc]), writes=(xB[dc],))
                                    if eg + 1 < NG:
                                        npc = ntt * 8
                                        lo = (dc * npc) // 32
                                        hi_ = ((dc + 1) * npc) // 32
                                        if hi_ > lo:
                                            emit_G(eg + 1, set(range(lo, hi_)))
                        items.append((v_h[ec][:, half * 2048:(half + 1) * 2048].rearrange("p (a b) -> p a b", a=4, b=512), (4, 512), cons_v))
            k.stream(items, LA=4)
            k.barrier()

    def ple_final(t0, N):
        with ExitStack() as ph:
            xn = sb(ph, "xn2", [128, 32, NT], BF16)
            xnB = bufs(32)
            gate = sb(ph, "gate", [128, 32, NT], BF16)
            gB = bufs(32)
            pTt = sb(ph, "pTt", [128, 2, NT], BF16)
            pB = Buf()
            tmp = [sb(ph, "ptmp%d" % i, [128, NT], F32) for i in range(2)]
            tmB = bufs(2)
            yo = [sb(ph, "yo%d" % i, [128, NT], F32) for i in range(4)]
            yoB = bufs(4)
            k.dma(pool, pTt[:, :, 0:N], pT_d[:, :, t0:t0 + N].rearrange("c p t -> p c t"), writes=(pB,))
            rmsnorm(N, GP, lambda c: (xn[:, c, 0:N], xnB[c], None))

            def evac_g(mc, pt, pb):
                k.op(act, lambda: nc.scalar.activation(out=gate[:, mc, 0:N], in_=pt[:, 0:N], func=AF.Sigmoid), reads=(pb,), writes=(gB[mc],))
            k.linear(w_pg_h, 32, 4, list(range(8)), lambda kc: (xn[:, kc, 0:N], xnB[kc]), N, evac_g)

            def evac_p(mc, pt, pb):
                t_, tB_ = tmp[mc % 2], tmB[mc % 2]
                k.op(dve, lambda: nc.vector.tensor_tensor(out=t_[:, 0:N], in0=pt[:, 0:N], in1=gate[:, mc, 0:N], op=ALU.mult), reads=(pb, gB[mc]), writes=(tB_,))
                k.op(dve, lambda: nc.vector.tensor_tensor(out=xT[:, mc, 0:N], in0=t_[:, 0:N], in1=xT[:, mc, 0:N], op=ALU.add), reads=(tB_, xB[mc]), writes=(xB[mc],))
            k.linear(w_ple_h, 2, 2, list(range(8)), lambda kc: (pTt[:, kc, 0:N], pB), N, evac_p)

            def emit(c):
                def post(c):
                    k.dma(sp, yT_d[c, :, t0:t0 + N], yo[c % 4][:, 0:N], reads=(yoB[c % 4],))
                return yo[c % 4][:, 0:N], yoB[c % 4], post
            rmsnorm(N, GL, emit)
            k.barrier()

    k.barrier()
    k.barrier()
    mixer_pre_all()
    load_cz()
    mixer("halo", xpre_d, PRE - 128, 128)
    nmain = SEG // NT
    for i in range(nmain):
        mixer("main", xT_d, i * NT, NT, out_idx=(0 if i == nmain - 1 else None))
        peer(NT)
        ple_final(i * NT, NT)
    mixer("sample", xT_d, SEG, NSAMP)
    peer(NSAMP)
    ple_final(SEG, NSAMP)
    for so in k.allsems:
        v = so.total
        if v > 0:
            sp.obj.wait_ge(so.h, v)
    k.barrier()
    es.close()
    return nc


def _fm(a):
    T, Fd = a.shape
    return np.ascontiguousarray(a.T.reshape(Fd // 128, 128, T))


def _blk(W, KBk=4):
    K, M = W.shape
    KC = K // 128
    KG = KC // KBk
    MG = M // 512
    return np.ascontiguousarray(W.reshape(KG, KBk, 128, MG, 512).transpose(3, 0, 2, 1, 4))


_NC_CACHE = {}


def kernel(x_prompt, x_sample, p_prompt, p_sample, cache_pool, state_ssm_re, state_ssm_im,
           g_mix, w_in, w_pool, pool_scale, ssm_a_re, ssm_a_im, ssm_log_dt, ssm_b_re, ssm_b_im,
           ssm_c_re, ssm_c_im, ssm_d, w_glu, b_glu, w_out, g_ffn, w_query, peer_sub_keys,
           expert_u, expert_v, g_ple, w_ple_gate, w_ple, g_final):
    f = np.float32
    A = lambda a: np.asarray(a, dtype=f)
    x_prompt, x_sample, p_prompt, p_sample = A(x_prompt), A(x_sample), A(p_prompt), A(p_sample)
    cache_pool, state_ssm_re, state_ssm_im = A(cache_pool), A(state_ssm_re), A(state_ssm_im)
    shared = {}
    vecs = np.zeros((128, 176), f)
    for off, g in ((0, g_mix), (32, g_ffn), (64, g_ple), (96, g_final)):
        vecs[:, off:off + 32] = A(g).reshape(32, 128).T
    vecs[:, 128:144] = A(pool_scale).reshape(16, 128).T
    vecs[:, 144:160] = A(b_glu).reshape(16, 128).T
    vecs[:, 160:176] = A(ssm_d).reshape(16, 128).T
    shared["vecs"] = vecs
    shared["ident"] = np.eye(128, dtype=f)
    rmask = np.zeros((128, 4), f)
    for q in range(4):
        rmask[q * 32:(q + 1) * 32, q] = 1.0
    shared["rmask"] = rmask
    are, aim, ldt = A(ssm_a_re)[0], A(ssm_a_im)[0], A(ssm_log_dt)[0]
    ldt_full = np.repeat(ldt[:, None], 64, axis=1)
    lamp = np.zeros((128, 3, NLAM), f)
    for i, a in enumerate((are, aim, ldt_full)):
        lamp[:, i, 0:64] = a.reshape(64, 2, 64).transpose(1, 2, 0).reshape(128, 64)
        a4 = a.reshape(16, 4, 2, 64)
        l2 = a4.transpose(1, 0, 2, 3).reshape(4, 16 * 128)
        lamp[:, i, 64:] = np.repeat(l2, 32, axis=0)
    shared["lamp"] = lamp
    bq = np.zeros((2, 128, 16, 128), f)
    for ri, b in enumerate((A(ssm_b_re)[0], A(ssm_b_im)[0])):
        b6 = b.reshape(16, 4, 2, 64, 16)
        for e in range(2):
            blk = b6[:, :, e].transpose(1, 3, 0, 2)
            bq[ri].reshape(4, 2, 16, 16, 2, 64)[:, e, :, :, e, :] = blk
    shared["bq"] = bq
    cz = np.zeros((2, 128, 64, 128), f)
    for ri, cc in enumerate((A(ssm_c_re)[0], A(ssm_c_im)[0])):
        c5 = cc.reshape(64, 2, 16, 64)
        czv = cz[ri].reshape(2, 64, 64, 8, 16)
        for gj in range(64):
            for e in range(2):
                czv[e, :, gj, (2 * gj + e) % 8, :] = c5[gj, e].T
    shared["cz"] = cz
    shared["keysT"] = np.ascontiguousarray(A(peer_sub_keys)[0].reshape(16, 128, 128).transpose(2, 0, 1))
    shared["w_in_b"] = _blk(A(w_in)[0])
    shared["w_pool_b"] = np.stack([_blk(A(w_pool)[0, g])[0] for g in range(4)])
    shared["w_glu_b"] = _blk(A(w_glu)[0])
    shared["w_out_b"] = _blk(A(w_out)[0])
    shared["w_q_b"] = _blk(A(w_query)[0])
    shared["w_pg_b"] = _blk(A(w_ple_gate)[0])
    shared["w_ple_b"] = _blk(A(w_ple)[0], 2)
    shared["UTb"] = np.ascontiguousarray(A(expert_u)[0].reshape(128, 128, 2, 16, 128).transpose(0, 2, 4, 3, 1))
    shared["Vn"] = np.ascontiguousarray(A(expert_v)[0].reshape(128, 128, 4096))

    in_maps = []
    for c in range(NCORES):
        b, kq = c // 4, c % 4
        m = dict(shared)
        xs = np.concatenate([x_prompt[b, kq * SEG:(kq + 1) * SEG], x_sample[2 * c], x_sample[2 * c + 1]], axis=0)
        m["xT"] = _fm(xs)
        xp = np.zeros((PRE, D), f)
        if kq > 0:
            xp[PRE - kq * SEG:] = x_prompt[b, 0:kq * SEG]
        m["xpreT"] = _fm(xp)
        ps_ = np.concatenate([p_prompt[0, b, kq * SEG:(kq + 1) * SEG], p_sample[0, 2 * c], p_sample[0, 2 * c + 1]], axis=0)
        m["pT"] = _fm(ps_)
        cp = np.zeros((2, 128, 16, 16), f)
        st0 = np.zeros((2, 128, 2, 64), f)
        for s in range(2):
            cpad = np.concatenate([np.zeros((1, 2048), f), cache_pool[0, 2 * c + s]], axis=0)
            cp[s] = cpad.T.reshape(16, 128, 16).transpose(1, 0, 2)
            for ri, stt in enumerate((state_ssm_re, state_ssm_im)):
                st0[s, :, ri, :] = stt[0, 2 * c + s].reshape(64, 2, 64).transpose(1, 2, 0).reshape(128, 64)
        m["cpT"] = cp
        m["st0"] = st0
        rc = np.ones((128, 4, 16), f)
        for g in range(4):
            w = 2 << g
            for t in range(16):
                pos = kq * SEG + t
                rc[:, g, t] = w / min(pos + 1, w)
        m["rcorr"] = rc
        in_maps.append(m)

    if "nc" not in _NC_CACHE:
        _NC_CACHE["nc"] = build_program()
    nc = _NC_CACHE["nc"]
    res = run_bass_kernel_spmd(nc, in_maps, core_ids=list(range(NCORES)))
    R = res.results

    def unfm(yT):
        return yT.reshape(4096, -1).T

    y_prompt = np.zeros((2, 8192, D), f)
    y_sample = np.zeros((16, 64, D), f)
    new_pool_p = np.zeros((1, 2, 15, 2048), f)
    new_re_p = np.zeros((1, 2, 128, 64), f)
    new_im_p = np.zeros((1, 2, 128, 64), f)
    new_pool_s = np.zeros((1, 16, 15, 2048), f)
    new_re_s = np.zeros((1, 16, 128, 64), f)
    new_im_s = np.zeros((1, 16, 128, 64), f)

    def unpool(po):
        return po.transpose(1, 0, 2).reshape(2048, 16).T[1:]

    def unst(so):
        return so.reshape(2, 64, 64).transpose(2, 0, 1).reshape(128, 64)

    for c in range(NCORES):
        b, kq = c // 4, c % 4
        y = unfm(R[c]["yT"])
        y_prompt[b, kq * SEG:(kq + 1) * SEG] = y[0:SEG]
        y_sample[2 * c] = y[SEG:SEG + 64]
        y_sample[2 * c + 1] = y[SEG + 64:SEG + 128]
        po, so = R[c]["pool_o"], R[c]["ssm_o"]
        if kq == 3:
            new_pool_p[0, b] = unpool(po[0])
            new_re_p[0, b] = unst(so[0][:, 0, :])
            new_im_p[0, b] = unst(so[0][:, 1, :])
        for s in range(2):
            new_pool_s[0, 2 * c + s] = unpool(po[1 + s])
            new_re_s[0, 2 * c + s] = unst(so[1 + s][:, 0, :])
            new_im_s[0, 2 * c + s] = unst(so[1 + s][:, 1, :])
    return (y_prompt, y_sample, new_pool_p, new_re_p, new_im_p, new_pool_s, new_re_s, new_im_s)
```
